# Optimizing a Trainium2 kernel written in Bass

```python
import math
import jax
import jax.numpy as jnp
from jax import lax
import numpy as np

D_MODEL = 1024
BATCH = 16
SEQ = 2048
DEPTH = 2

N_MIXERS = 2
N_NSA_LAYERS = (DEPTH + 1) // 2
N_HGRN_LAYERS = DEPTH // 2

NSA_HEADS = 16
NSA_KV_GROUPS = 4
NSA_GROUP_SIZE = NSA_HEADS // NSA_KV_GROUPS
NSA_HEAD_DIM = D_MODEL // NSA_HEADS
NSA_Q_DIM = NSA_HEADS * NSA_HEAD_DIM
NSA_KV_DIM = NSA_KV_GROUPS * NSA_HEAD_DIM
NSA_IN_DIM = NSA_Q_DIM + 6 * NSA_KV_DIM + 3 * NSA_HEADS
CMP_BLOCK = 32
CMP_STRIDE = 16
SEL_BLOCK = 64
N_SELECT = 8
WINDOW = 512
Q_BLOCK = 32
FORCED_SCORE = 1.0e4

REL_BUCKETS = 32
REL_MAX_DISTANCE = 1024

HGRN_EXPAND = 128
HGRN_HEADS = D_MODEL // HGRN_EXPAND
HGRN_KEY_DIM = HGRN_EXPAND
HGRN_VALUE_DIM = D_MODEL // HGRN_HEADS
HGRN_F_DIM = HGRN_HEADS * HGRN_KEY_DIM
HGRN_IN_DIM = 2 * HGRN_F_DIM + 2 * D_MODEL
HGRN_CHUNK = 32

MOE_GROUPS = 4
MOE_EXPERTS_PER_GROUP = 4
MOE_EXPERTS = MOE_GROUPS * MOE_EXPERTS_PER_GROUP
MOE_TOP_K = 2
MOE_FF = 512
MOE_ROW_BLOCK = 128

NORM_EPS = 1e-6

kernel_name = 'hybrid_nsa_hgrn2_hier_moe_adaln'


def rms_norm(x, gain):
    xf = x.astype(jnp.float32)
    y = xf * lax.rsqrt(jnp.mean(xf * xf, axis=-1, keepdims=True) + NORM_EPS)
    return (y * gain.astype(jnp.float32)).astype(x.dtype)


def masked_softmax(logits, mask):
    logits = jnp.where(mask, logits, -jnp.inf)
    m = jnp.max(logits, axis=-1, keepdims=True)
    m = jnp.where(jnp.isfinite(m), m, 0.0)
    e = jnp.exp(logits - m)
    return e / jnp.maximum(jnp.sum(e, axis=-1, keepdims=True), 1e-30)


def t5_bucket(dist):
    n = jnp.maximum(dist, 0)
    max_exact = REL_BUCKETS // 2
    nf = jnp.maximum(n, 1).astype(jnp.float32)
    large = max_exact + (jnp.log(nf / max_exact) / math.log(REL_MAX_DISTANCE / max_exact)
                         * (REL_BUCKETS - max_exact)).astype(jnp.int32)
    large = jnp.minimum(large, REL_BUCKETS - 1)
    return jnp.where(n < max_exact, n, large)


def compress_blocks(kv, pe, w1, w2):
    b, t, g, d = kv.shape
    n_cmp = (t - CMP_BLOCK) // CMP_STRIDE + 1
    idx = np.arange(n_cmp)[:, None] * CMP_STRIDE + np.arange(CMP_BLOCK)[None, :]
    blocks = kv[:, idx] + pe[None, None, :, None, :]
    flat = blocks.transpose(0, 1, 3, 2, 4).reshape(b, n_cmp, g, CMP_BLOCK * d)
    return jax.nn.silu(flat @ w1) @ w2


def cmp_to_sel_weights(n_cmp, n_sel):
    cs = np.arange(n_cmp) * CMP_STRIDE
    ss = np.arange(n_sel) * SEL_BLOCK
    shared = (np.minimum(cs[:, None] + CMP_BLOCK, ss[None, :] + SEL_BLOCK)
              - np.maximum(cs[:, None], ss[None, :]))
    return jnp.asarray(np.clip(shared, 0, None) / CMP_BLOCK, dtype=jnp.float32)


def nsa_mixer(h, rel_bias, w_in, q_gain, k_gain, cmp_pe, cmp_w1, cmp_w2, w_out):
    b, t, _ = h.shape
    G, R, Dh = NSA_KV_GROUPS, NSA_GROUP_SIZE, NSA_HEAD_DIM
    cuts = [NSA_Q_DIM + i * NSA_KV_DIM for i in range(7)]
    q, kc, vc, ks, vs, kw, vw, gl = jnp.split(h @ w_in, cuts, axis=-1)
    kvs = (b, t, G, Dh)
    q = rms_norm(q.reshape(b, t, G, R, Dh), q_gain)
    kc = rms_norm(compress_blocks(kc.reshape(kvs), cmp_pe[0], cmp_w1[0], cmp_w2[0]), k_gain[0])
    vc = compress_blocks(vc.reshape(kvs), cmp_pe[1], cmp_w1[1], cmp_w2[1])
    ks = rms_norm(ks.reshape(kvs), k_gain[1])
    vs = vs.reshape(kvs)
    kw = rms_norm(kw.reshape(kvs), k_gain[2])
    vw = vw.reshape(kvs)
    gates = jax.nn.sigmoid(gl.astype(jnp.float32)).reshape(b, t, G, R, 3)

    n_cmp = kc.shape[1]
    n_sel_blocks = t // SEL_BLOCK
    n_select = min(N_SELECT, n_sel_blocks)
    n_keys_sel = n_select * SEL_BLOCK
    cmp_end = jnp.arange(n_cmp) * CMP_STRIDE + (CMP_BLOCK - 1)
    cmp_to_sel = cmp_to_sel_weights(n_cmp, n_sel_blocks)
    ks_blk = ks.transpose(0, 2, 1, 3).reshape(b, G, n_sel_blocks, SEL_BLOCK, Dh)
    vs_blk = vs.transpose(0, 2, 1, 3).reshape(b, G, n_sel_blocks, SEL_BLOCK, Dh)
    kw_pad = jnp.pad(kw, ((0, 0), (WINDOW, 0), (0, 0), (0, 0)))
    vw_pad = jnp.pad(vw, ((0, 0), (WINDOW, 0), (0, 0), (0, 0)))
    bias_tbl = rel_bias.astype(jnp.float32).T.reshape(G, R, REL_BUCKETS)
    scale = NSA_HEAD_DIM ** -0.5
    blk_ids = jnp.arange(n_sel_blocks)
    b_ix = jnp.arange(b)[:, None, None, None]
    g_ix = jnp.arange(G)[None, :, None, None]
    g_ix5 = jnp.arange(G)[None, :, None, None, None]
    r_ix5 = jnp.arange(R)[None, None, :, None, None]

    def query_block(qb):
        t0 = qb * Q_BLOCK
        tq = t0 + jnp.arange(Q_BLOCK)
        q_b = lax.dynamic_slice_in_dim(q, t0, Q_BLOCK, axis=1)
        gate_b = lax.dynamic_slice_in_dim(gates, t0, Q_BLOCK, axis=1)

        dist_c = tq[:, None] - cmp_end[None, :]
        lg = (jnp.einsum('bqgrd,bngd->bgrqn', q_b, kc).astype(jnp.float32) * scale
              + bias_tbl[:, :, t5_bucket(dist_c)])
        p_c = masked_softmax(lg, dist_c >= 0)
        o_c = jnp.einsum('bgrqn,bngd->bqgrd', p_c.astype(vc.dtype), vc)

        imp = jnp.einsum('bgrqn,ns->bgqs', p_c, cmp_to_sel)
        cur = tq // SEL_BLOCK
        forced = ((blk_ids[None, :] == 0) | (blk_ids[None, :] == cur[:, None])
                  | (blk_ids[None, :] == cur[:, None] - 1))
        causal_blk = blk_ids[None, :] <= cur[:, None]
        score = jnp.where(forced, FORCED_SCORE, jnp.where(causal_blk, imp, -1.0))
        _, sel = lax.top_k(score, n_select)
        k_sel = ks_blk[b_ix, g_ix, sel].reshape(b, G, Q_BLOCK, n_keys_sel, Dh)
        v_sel = vs_blk[b_ix, g_ix, sel].reshape(b, G, Q_BLOCK, n_keys_sel, Dh)
        pos = (sel[..., None] * SEL_BLOCK + jnp.arange(SEL_BLOCK)).reshape(b, G, Q_BLOCK, n_keys_sel)
        dist_s = tq[None, None, :, None] - pos
        lg = (jnp.einsum('bqgrd,bgqkd->bgrqk', q_b, k_sel).astype(jnp.float32) * scale
              + bias_tbl[g_ix5, r_ix5, t5_bucket(dist_s)[:, :, None]])
        p_s = masked_softmax(lg, (dist_s >= 0)[:, :, None])
        o_s = jnp.einsum('bgrqk,bgqkd->bqgrd', p_s.astype(v_sel.dtype), v_sel)

        k_w = lax.dynamic_slice_in_dim(kw_pad, t0, Q_BLOCK + WINDOW, axis=1)
        v_w = lax.dynamic_slice_in_dim(vw_pad, t0, Q_BLOCK + WINDOW, axis=1)
        kpos = t0 - WINDOW + jnp.arange(Q_BLOCK + WINDOW)
        dist_w = tq[:, None] - kpos[None, :]
        mask_w = (dist_w >= 0) & (dist_w < WINDOW) & (kpos[None, :] >= 0)
        lg = (jnp.einsum('bqgrd,bkgd->bgrqk', q_b, k_w).astype(jnp.float32) * scale
              + bias_tbl[:, :, t5_bucket(dist_w)])
        p_w = masked_softmax(lg, mask_w)
        o_w = jnp.einsum('bgrqk,bkgd->bqgrd', p_w.astype(v_w.dtype), v_w)

        o = gate_b[..., 0:1] * o_c + gate_b[..., 1:2] * o_s + gate_b[..., 2:3] * o_w
        return o.astype(h.dtype).reshape(b, Q_BLOCK, NSA_Q_DIM)

    o = lax.map(query_block, jnp.arange(t // Q_BLOCK))
    o = o.transpose(1, 0, 2, 3).reshape(b, t, NSA_Q_DIM)
    return o @ w_out


def hgrn2_mixer(h, lower_bound, w_in, out_gain, w_out):
    b, t, _ = h.shape
    NH, dk, dv, C = HGRN_HEADS, HGRN_KEY_DIM, HGRN_VALUE_DIM, HGRN_CHUNK
    q, f, i, g = jnp.split(h @ w_in, [HGRN_F_DIM, 2 * HGRN_F_DIM, 2 * HGRN_F_DIM + D_MODEL], axis=-1)
    q = jax.nn.silu(q.astype(jnp.float32)).reshape(b, t, NH, dk)
    lb = lower_bound.astype(jnp.float32).reshape(NH, dk)
    log_f = jnp.logaddexp(jnp.log(lb), jnp.log1p(-lb)
                          + jax.nn.log_sigmoid(f.astype(jnp.float32).reshape(b, t, NH, dk)))
    k = -jnp.expm1(log_f)
    v = i.astype(jnp.float32).reshape(b, t, NH, dv)
    n_chunks = t // C

    def to_chunks(a):
        return a.reshape(b, n_chunks, C, NH, a.shape[-1]).transpose(1, 0, 3, 2, 4)

    causal = jnp.tril(jnp.ones((C, C), dtype=bool))

    def chunk_step(S, xs):
        q_c, k_c, v_c, lf_c = xs
        cum = jnp.cumsum(lf_c, axis=2)
        rel = jnp.where(causal[:, :, None], cum[:, :, :, None, :] - cum[:, :, None, :, :], -jnp.inf)
        attn = jnp.einsum('bhtd,bhsd,bhtsd->bhts', q_c, k_c, jnp.exp(rel))
        o = attn @ v_c + jnp.einsum('bhtd,bhde->bhte', q_c * jnp.exp(cum), S)
        last = cum[:, :, -1:, :]
        S = (jnp.exp(last[:, :, 0, :, None]) * S
             + jnp.einsum('bhsd,bhse->bhde', k_c * jnp.exp(last - cum), v_c))
        return S, o

    S0 = jnp.zeros((b, NH, dk, dv), jnp.float32)
    _, o = lax.scan(chunk_step, S0, (to_chunks(q), to_chunks(k), to_chunks(v), to_chunks(log_f)))
    o = o.transpose(1, 0, 3, 2, 4).reshape(b, t, NH, dv)
    o = rms_norm(o, out_gain) * jax.nn.silu(g.astype(jnp.float32).reshape(b, t, NH, dv))
    return o.reshape(b, t, D_MODEL).astype(h.dtype) @ w_out


def grouped_expert_ffn(tok, expert, weight, w1, w3, w2):
    n, d = tok.shape
    a = n * MOE_TOP_K
    flat_e = expert.reshape(a)
    flat_t = jnp.repeat(jnp.arange(n, dtype=jnp.int32), MOE_TOP_K)
    flat_w = weight.reshape(a)
    order = jnp.argsort(flat_e)
    e_sorted, t_sorted, w_sorted = flat_e[order], flat_t[order], flat_w[order]
    counts = jnp.bincount(flat_e, length=MOE_EXPERTS)
    padded = (counts + MOE_ROW_BLOCK - 1) // MOE_ROW_BLOCK * MOE_ROW_BLOCK
    pad_end = jnp.cumsum(padded)
    pad_start = pad_end - padded
    start = jnp.cumsum(counts) - counts
    dest = pad_start[e_sorted] + (jnp.arange(a) - start[e_sorted])
    n_blk = -(-(a + MOE_EXPERTS * (MOE_ROW_BLOCK - 1)) // MOE_ROW_BLOCK)
    p = n_blk * MOE_ROW_BLOCK
    buf_t = jnp.full((p,), n, dtype=jnp.int32).at[dest].set(t_sorted)
    buf_w = jnp.zeros((p,), tok.dtype).at[dest].set(w_sorted)
    blk_e = jnp.minimum(jnp.searchsorted(pad_end, jnp.arange(n_blk) * MOE_ROW_BLOCK, side='right'),
                        MOE_EXPERTS - 1)
    tok_pad = jnp.concatenate([tok, jnp.zeros((1, d), tok.dtype)], axis=0)
    xb = tok_pad[buf_t].reshape(n_blk, MOE_ROW_BLOCK, d)

    def expert_block(args):
        xblk, e = args
        return (jax.nn.silu(xblk @ w1[e]) * (xblk @ w3[e])) @ w2[e]

    yb = lax.map(expert_block, (xb, blk_e)).reshape(p, d)
    out = jnp.zeros((n + 1, d), tok.dtype).at[buf_t].add(yb * buf_w[:, None])
    return out[:n]


def hier_moe(h, w_router_group, w_router_expert, w1, w3, w2):
    b, t, d = h.shape
    n = b * t
    tok = h.reshape(n, d)
    p_group = jax.nn.softmax((tok @ w_router_group).astype(jnp.float32), axis=-1)
    grp = jnp.argmax(p_group, axis=-1).astype(jnp.int32)
    p_grp_sel = jnp.take_along_axis(p_group, grp[:, None], axis=-1)
    e_logits = (tok @ w_router_expert).astype(jnp.float32).reshape(n, MOE_GROUPS, MOE_EXPERTS_PER_GROUP)
    e_logits = jnp.take_along_axis(e_logits, grp[:, None, None], axis=1)[:, 0]
    top_p, top_i = lax.top_k(jax.nn.softmax(e_logits, axis=-1), MOE_TOP_K)
    weight = p_grp_sel * top_p / jnp.sum(top_p, axis=-1, keepdims=True)
    expert = grp[:, None] * MOE_EXPERTS_PER_GROUP + top_i.astype(jnp.int32)
    y = grouped_expert_ffn(tok, expert, weight.astype(h.dtype), w1, w3, w2)
    return y.reshape(b, t, d)


def setup_inputs(seed: int = 0) -> dict:
    key = jax.random.key(seed)
    ks = jax.random.split(key, 24)
    f32 = jnp.float32
    D = D_MODEL

    def nrm(k, shape, s):
        return jax.random.normal(k, shape, f32) * s

    return {
        'x': nrm(ks[0], (BATCH, SEQ, D), 1.0),
        'c': nrm(ks[1], (BATCH, D), 1.0),
        'ada_w': nrm(ks[2], (DEPTH, 2, D, 3 * D), 0.5 * D ** -0.5),
        'ada_b': nrm(ks[3], (DEPTH, 2, 3 * D), 0.01),
        'norm_g': 1.0 + nrm(ks[4], (DEPTH, 2, D), 0.01),
        'rel_bias': nrm(ks[5], (REL_BUCKETS, NSA_HEADS), 0.5),
        'nsa_w_in': nrm(ks[6], (N_NSA_LAYERS, D, NSA_IN_DIM), D ** -0.5),
        'nsa_q_gain': 1.0 + nrm(ks[7], (N_NSA_LAYERS, NSA_HEAD_DIM), 0.01),
        'nsa_k_gain': 1.0 + nrm(ks[8], (N_NSA_LAYERS, 3, NSA_HEAD_DIM), 0.01),
        'nsa_cmp_pe': nrm(ks[9], (N_NSA_LAYERS, 2, CMP_BLOCK, NSA_HEAD_DIM), 0.02),
        'nsa_cmp_w1': nrm(ks[10], (N_NSA_LAYERS, 2, CMP_BLOCK * NSA_HEAD_DIM, NSA_HEAD_DIM),
                          (CMP_BLOCK * NSA_HEAD_DIM) ** -0.5),
        'nsa_cmp_w2': nrm(ks[11], (N_NSA_LAYERS, 2, NSA_HEAD_DIM, NSA_HEAD_DIM), NSA_HEAD_DIM ** -0.5),
        'nsa_w_out': nrm(ks[12], (N_NSA_LAYERS, NSA_Q_DIM, D), NSA_Q_DIM ** -0.5),
        'hgrn_w_in': nrm(ks[13], (N_HGRN_LAYERS, D, HGRN_IN_DIM), D ** -0.5),
        'hgrn_lower_bounds': 1.0 + nrm(ks[14], (DEPTH, HGRN_F_DIM), 0.1),
        'hgrn_out_gain': 1.0 + nrm(ks[15], (N_HGRN_LAYERS, HGRN_VALUE_DIM), 0.01),
        'hgrn_w_out': nrm(ks[16], (N_HGRN_LAYERS, D, D), D ** -0.5),
        'moe_router_group': nrm(ks[17], (DEPTH, D, MOE_GROUPS), D ** -0.5),
        'moe_router_expert': nrm(ks[18], (DEPTH, D, MOE_EXPERTS), D ** -0.5),
        'moe_w1': nrm(ks[19], (DEPTH, MOE_EXPERTS, D, MOE_FF), D ** -0.5),
        'moe_w3': nrm(ks[20], (DEPTH, MOE_EXPERTS, D, MOE_FF), D ** -0.5),
        'moe_w2': nrm(ks[21], (DEPTH, MOE_EXPERTS, MOE_FF, D), MOE_FF ** -0.5),
    }


def reference(x, c, ada_w, ada_b, norm_g, rel_bias, nsa_w_in, nsa_q_gain, nsa_k_gain,
              nsa_cmp_pe, nsa_cmp_w1, nsa_cmp_w2, nsa_w_out, hgrn_w_in, hgrn_lower_bounds,
              hgrn_out_gain, hgrn_w_out, moe_router_group, moe_router_expert,
              moe_w1, moe_w3, moe_w2):
    lb_soft = jax.nn.softmax(hgrn_lower_bounds.astype(jnp.float32), axis=0)
    lb_all = jnp.cumsum(lb_soft, axis=0) - lb_soft[0]
    cond = jax.nn.silu(c)
    for layer in range(DEPTH):
        mod = jnp.einsum('bd,sde->sbe', cond, ada_w[layer]) + ada_b[layer][:, None, :]
        j = layer // N_MIXERS

        shift, scale, gate = jnp.split(mod[0], 3, axis=-1)
        h = rms_norm(x, norm_g[layer, 0]) * (1.0 + scale[:, None, :]) + shift[:, None, :]
        if layer % N_MIXERS == 0:
            y = nsa_mixer(h, rel_bias, nsa_w_in[j], nsa_q_gain[j], nsa_k_gain[j],
                          nsa_cmp_pe[j], nsa_cmp_w1[j], nsa_cmp_w2[j], nsa_w_out[j])
        else:
            y = hgrn2_mixer(h, lb_all[layer], hgrn_w_in[j], hgrn_out_gain[j], hgrn_w_out[j])
        x = x + gate[:, None, :] * y

        shift, scale, gate = jnp.split(mod[1], 3, axis=-1)
        h = rms_norm(x, norm_g[layer, 1]) * (1.0 + scale[:, None, :]) + shift[:, None, :]
        y = hier_moe(h, moe_router_group[layer], moe_router_expert[layer],
                     moe_w1[layer], moe_w3[layer], moe_w2[layer])
        x = x + gate[:, None, :] * y
    return x
```

```python
from contextlib import ExitStack
import numpy as np
import concourse.bass as bass
import concourse.mybir as mybir
from concourse.ap import AP
from concourse.bass_utils import run_bass_kernel_spmd

F32 = mybir.dt.float32
BF16 = mybir.dt.bfloat16
I32 = mybir.dt.int32
ALU = mybir.AluOpType
AF = mybir.ActivationFunctionType
AX = mybir.AxisListType

ENGS = ["pe", "act", "dve", "pool", "sp"]


class Res:
    __slots__ = ("name", "w", "r", "xw", "excl")

    def __init__(self, name=""):
        self.name = name
        self.excl = False
        self.xw = {}
        self.w = {}
        self.r = {}


class T:
    def __init__(self, h, res=None, name=""):
        self.h = h
        self.res = res if res is not None else Res(name)

    def __getitem__(self, k):
        return self.h[k]

    def ap(self):
        return self.h.ap() if hasattr(self.h, "ap") and callable(self.h.ap) else self.h[:]


def _res(x):
    if isinstance(x, T):
        return x.res
    return x


class Sch:
    def __init__(self, nc, n_dma_sems=28, same_engine_sync=True):
        self.nc = nc
        self.stack = ExitStack()
        self.items = {e: [] for e in ENGS}
        self.count = {e: 0 for e in ENGS}
        self.esem = {e: self.stack.enter_context(nc.semaphore(f"s_{e}")) for e in ENGS}
        self.dsem = [self.stack.enter_context(nc.semaphore(f"sd{i}")) for i in range(n_dma_sems)]
        self.dcount = [0] * n_dma_sems
        self.n_hw = 16
        self.dnext = {"hw": 0, "sw": 0}
        self.seen = {e: {} for e in ENGS}
        self.same = same_engine_sync
        self.nid = 0
        self.SB_WORDS = 53000
        self.big = self.stack.enter_context(nc.sbuf_tensor("bigsb", [128, self.SB_WORDS], F32))
        self.sb_off = 0
        self.sb_peak = 0

    def sb(self, shape, dtype=F32, name=None):
        shape = list(shape)
        n = int(np.prod(shape[1:]))
        n_al = (n + 7) // 8 * 8
        assert self.sb_off + n_al <= self.SB_WORDS, f"SBUF carve overflow at {name}: {self.sb_off}+{n_al}"
        v = self.big[0:shape[0], self.sb_off:self.sb_off + n]
        self.sb_off += n_al
        self.sb_peak = max(self.sb_peak, self.sb_off)
        if dtype != F32:
            v = v.bitcast(dtype)
        if len(shape) == 3:
            v = v.rearrange("p (a b) -> p a b", b=shape[2])
        elif len(shape) == 4:
            v = v.rearrange("p (a b c) -> p a b c", b=shape[2], c=shape[3])
        return T(v, name=name or "t")

    def mark(self):
        return self.sb_off

    def release(self, mark):
        self.barrier()
        self.sb_off = mark

    def barrier(self):
        for eng in ENGS:
            waits = []
            seen = self.seen[eng]
            for i, c in enumerate(self.dcount):
                if c and seen.get(("d", i), 0) < 16 * c:
                    seen[("d", i)] = 16 * c
                    waits.append((self.dsem[i], 16 * c))
            for e in ENGS:
                if e != eng and self.count[e] and seen.get(("e", e), 0) < self.count[e]:
                    seen[("e", e)] = self.count[e]
                    waits.append((self.esem[e], self.count[e]))
            if waits:
                self.items[eng].append((waits, None, None, 0))

    def ps(self, shape, dtype=F32, name=None):
        self.nid += 1
        name = name or f"p{self.nid}"
        h = self.stack.enter_context(self.nc.psum_tensor(f"{name}_{self.nid}", list(shape), dtype))
        t = T(h, name=name)
        t.res.excl = True
        return t

    def dram(self, name, shape, dtype=F32, kind="Internal"):
        h = self.nc.dram_tensor(name, list(shape), dtype, kind=kind)
        return T(h, name=name)

    def _sem(self, key):
        return self.esem[key[1]] if key[0] == "e" else self.dsem[key[1]]

    def _deps(self, eng, reads, writes, conc=False):
        deps = {}

        def add(tok):
            if tok is None:
                return
            key, val = tok
            if key[0] == "e" and key[1] == eng:
                if eng == "pe" or not self.same:
                    return
            if deps.get(key, 0) < val:
                deps[key] = val

        for r in reads:
            for key, val in _res(r).w.items():
                add((key, val))
        for w in writes:
            rr = _res(w)
            for key, val in (rr.xw if conc else rr.w).items():
                add((key, val))
            for key, val in rr.r.items():
                add((key, val))
        waits = []
        seen = self.seen[eng]
        for key, val in deps.items():
            if seen.get(key, 0) >= val:
                continue
            seen[key] = val
            waits.append((self._sem(key), val))
        return waits

    def _commit(self, tok, reads, writes, conc=False):
        key, val = tok
        for r in reads:
            rr = _res(r)
            if rr.r.get(key, 0) < val:
                rr.r[key] = val
        for w in writes:
            rr = _res(w)
            if conc:
                if rr.w.get(key, 0) < val:
                    rr.w[key] = val
            else:
                rr.w = {key: val}
                rr.xw = {key: val}
                rr.r = {}

    def op(self, eng, fn, reads=(), writes=(), conc=False):
        ex = [r for r in reads if _res(r).excl]
        if ex:
            assert not conc or all(not _res(w).excl for w in writes)
            if conc:
                w0 = self._deps(eng, [], ex, False)
                self.items[eng].append((w0, None, None, 0)) if w0 else None
                reads = [r for r in reads if not _res(r).excl]
                exq = ex
            else:
                writes = list(writes) + ex
                reads = [r for r in reads if not _res(r).excl]
                exq = []
        else:
            exq = []
        waits = self._deps(eng, reads, writes, conc)
        self.count[eng] += 1
        tok = (("e", eng), self.count[eng])
        self.items[eng].append((waits, fn, self.esem[eng], 1))
        self._commit(tok, reads, writes, conc)
        if exq:
            self._commit(tok, [], exq, False)
        return tok

    def dma(self, eng, fn, reads=(), writes=(), conc=False):
        waits = self._deps(eng, reads, writes, conc)
        if eng == "pool":
            i = self.n_hw + self.dnext["sw"]
            self.dnext["sw"] = (self.dnext["sw"] + 1) % (len(self.dsem) - self.n_hw)
        else:
            i = self.dnext["hw"]
            self.dnext["hw"] = (self.dnext["hw"] + 1) % self.n_hw
        if self.dcount[i] > 0:
            key = ("d", i)
            val = 16 * self.dcount[i]
            if self.seen[eng].get(key, 0) < val:
                self.seen[eng][key] = val
                waits.append((self.dsem[i], val))
        self.dcount[i] += 1
        tok = (("d", i), 16 * self.dcount[i])
        self.items[eng].append((waits, fn, self.dsem[i], 16))
        self._commit(tok, reads, writes, conc)
        return tok

    def load(self, dst_ap, src_ap, reads=(), writes=(), eng="sp", conc=False, **kw):
        return self.dma(eng, lambda e: e.dma_start(out=dst_ap, in_=src_ap, **kw), reads, writes, conc)

    def finish(self, eng="sp"):
        waits = []
        for i, c in enumerate(self.dcount):
            if c:
                waits.append((self.dsem[i], 16 * c))
        for e in ENGS:
            if e != eng and self.count[e]:
                waits.append((self.esem[e], self.count[e]))
        self.items[eng].append((waits, None, None, 0))

    def emit(self):
        nc = self.nc
        items = self.items

        def replay(name, e):
            for waits, fn, sem, inc in items[name]:
                for s, v in waits:
                    e.wait_ge(s, v)
                if fn is not None:
                    ins = fn(e)
                    ins.then_inc(sem, inc)

        with nc.Block() as block:
            @block.sync
            def _(e):
                replay("sp", e)

            @block.tensor
            def _(e):
                replay("pe", e)

            @block.scalar
            def _(e):
                replay("act", e)

            @block.vector
            def _(e):
                replay("dve", e)

            @block.gpsimd
            def _(e):
                replay("pool", e)
        self.stack.close()


D = 1024
T_SEQ = 2048
NB = 2
NTOK = NB * T_SEQ
NT = NTOK // 128
TPS = T_SEQ // 128
EPS = 1e-6
NBLK = 80
N_CORES = 8


_BKT = {}


def t5_bucket_table(n):
    if n in _BKT:
        return _BKT[n]
    import math
    tab = None
    try:
        import jax
        import jax.numpy as jnp
        with jax.default_device(jax.devices("cpu")[0]):
            d = jnp.arange(n)
            nf = jnp.maximum(d, 1).astype(jnp.float32)
            large = 16 + (jnp.log(nf / 16) / math.log(1024 / 16) * 16).astype(jnp.int32)
            large = jnp.minimum(large, 31)
            tab = np.asarray(jnp.where(d < 16, d, large)).astype(np.int64)
    except Exception:
        tab = None
    if tab is None:
        d = np.arange(n)
        nf = np.maximum(d, 1).astype(np.float32)
        large = 16 + (np.log(nf / np.float32(16)) / np.float32(math.log(1024 / 16)) * np.float32(16)).astype(np.int32)
        large = np.minimum(large, 31)
        tab = np.where(d < 16, d, large).astype(np.int64)
    _BKT[n] = tab
    return tab


def host_consts():
    c = {}
    i = np.arange(128)
    c["ident"] = np.eye(128, dtype=np.float32)
    c["ones"] = np.ones((128, 128), np.float32)
    c["ustrict"] = (i[:, None] < i[None, :]).astype(np.float32)
    c["thr"] = np.tile((128.0 * np.arange(NBLK, dtype=np.float32))[None, :], (128, 1))
    c["piota"] = i.astype(np.float32).reshape(128, 1)
    seg = np.arange(2 * NT)
    tok = (seg[None, :] // 2) * 128 + i[:, None]
    c["tokid"] = tok.astype(np.int32)
    c["aid"] = (2 * tok + (seg[None, :] % 2)).astype(np.int32)
    init3 = np.zeros((128, NBLK, 3), np.int32)
    init3[:, :, 2] = 2 * NTOK + i[:, None] * NBLK + np.arange(NBLK)[None, :]
    c["init3"] = init3
    sel = np.zeros((2, 2, 128), np.float32)
    sel[0, 0, :] = 1.0
    sel[1, 1, :] = 1.0
    c["sel2"] = sel.reshape(2, 256)
    same = (i[:, None] // 32) == (i[None, :] // 32)
    c["ltri"] = (same & (i[:, None] <= i[None, :])).astype(np.float32)
    c["urev"] = (same & (i[:, None] > i[None, :])).astype(np.float32)
    cm = ((i[:, None] // 32) == np.arange(4)[None, :]).astype(np.float32)
    c["cm"] = cm
    c["cmrow"] = np.tile(cm.T.reshape(1, 512), (128, 1)).astype(np.float32)
    t = np.arange(TPS)[None, :, None] * 128 + i[:, None, None]
    cur = t // 64
    blk = np.arange(32)[None, None, :]
    forced = (blk == 0) | (blk == cur) | (blk == cur - 1)
    causal = blk <= cur
    c["cmnf"] = (causal & ~forced).astype(np.float32)
    c["sadd"] = (1.0e4 * forced - 1.0 * (~causal)).astype(np.float32)
    kt = np.arange(TPS)[None, :, None]
    c["ex"] = (((kt * 128 + np.arange(128)[None, None, :]) // 64) == np.arange(32)[:, None, None]).astype(np.float32)
    c["bkoh"] = (t5_bucket_table(T_SEQ)[None, :] == np.arange(32)[:, None]).astype(np.float32)
    cs = np.arange(127) * 16
    ss_ = np.arange(32) * 64
    shared = np.minimum(cs[:, None] + 32, ss_[None, :] + 64) - np.maximum(cs[:, None], ss_[None, :])
    c["c2s"] = (np.clip(shared, 0, None) / 32).astype(np.float32)
    return c


class Pool:
    def __init__(self, S, n, shape, dtype=F32, name="pool", psum=False, tiles=None):
        self.t = tiles if tiles is not None else [(S.ps if psum else S.sb)(shape, dtype, f"{name}{i}") for i in range(n)]
        self.i = 0

    def next(self):
        t = self.t[self.i]
        self.i = (self.i + 1) % len(self.t)
        return t


class Ctx:
    pass


LATE_CONSTS = ("cmnf", "sadd", "ex", "bkoh", "c2s", "sel2")
NEG = -30000.0
OOB_IDX = 65536.0


DEBUG = False


def dump(S, name, t, shape, dtype=F32):
    if not DEBUG:
        return
    d = S.dram("dbg_" + name, list(shape), dtype, kind="ExternalOutput")
    S.load(d.ap(), t[:], reads=[t], writes=[d])


def setup_common(S, C, ins):
    hc = host_consts()
    C.k = {}
    for name, arr in hc.items():
        if name in LATE_CONSTS:
            continue
        dt = I32 if arr.dtype == np.int32 else F32
        t = S.sb(list(arr.shape), dt, name)
        S.load(t[:], ins[name].ap(), reads=[ins[name]], writes=[t])
        C.k[name] = t
    C.psum = Pool(S, 8, [128, 512], F32, "bank", psum=True)
    cT = S.sb([128, 8, 2], F32, "cT")
    for b in range(2):
        S.load(cT[:, :, b], ins["c"][b, :].rearrange("(k p) -> p k", p=128), reads=[ins["c"]], writes=[cT],
               conc=(b > 0), allow_slow_non_contiguous=True)
    C.condT = S.sb([128, 8, 2], F32, "condT")
    S.op("act", lambda e: e.activation(out=C.condT[:], in_=cT[:], func=AF.Silu), reads=[cT], writes=[C.condT])
    C.G = [S.sb([128, 1024], F32, f"G{b}") for b in range(2)]
    C.Sh = [S.sb([128, 1024], F32, f"Sh{b}") for b in range(2)]
    C.Gt = [S.sb([128, 1024], F32, f"Gt{b}") for b in range(2)]
    C.xpool = Pool(S, 2, [128, 1024], F32, "xt")
    C.hpool = Pool(S, 2, [128, 1024], F32, "ht")
    C.sqpool = Pool(S, 1, [128, 1024], F32, "sq")
    C.hTpool = Pool(S, 2, [128, 8, 128], F32, "hT")
    C.stat = Pool(S, 4, [128, 2], F32, "stat")


def mod_tiles(S, C, ins, l, s):
    ada_w = ins["ada_w"]
    mk = S.mark()
    wpool = Pool(S, 2, [128, 8, 384], F32, "wmod")
    modT = S.sb([128, 24, 2], F32, "modT")
    bT = S.sb([128, 24], F32, "bT")
    gT = S.sb([128, 8], F32, "gT")
    dg = Pool(S, 2, [128, 128], F32, "diag")
    S.load(bT[:], ins["ada_b"][l, s, :].rearrange("(e p) -> p e", p=128), reads=[ins["ada_b"]], writes=[bT],
           allow_slow_non_contiguous=True)
    S.load(gT[:], ins["norm_g"][l, s, :].rearrange("(e p) -> p e", p=128), reads=[ins["norm_g"]], writes=[gT],
           allow_slow_non_contiguous=True)
    acc = C.psum.next()
    for cb in range(8):
        w = wpool.next()
        S.load(w[:], ada_w[l, s, :, cb * 384:(cb + 1) * 384].rearrange("(k p) n -> p k n", p=128), reads=[ada_w], writes=[w])
        for e in range(3):
            ee = cb * 3 + e
            for k in range(8):
                S.op("pe", lambda e_, e=e, ee=ee, k=k, w=w: e_.matmul(acc[:, ee * 2:ee * 2 + 2], w[:, k, e * 128:(e + 1) * 128], C.condT[:, k, :],
                                                                  start=(k == 0), stop=(k == 7)),
                     reads=[C.condT, w], writes=[acc])
    S.op("dve", lambda e: e.tensor_tensor(out=modT[:], in0=acc[:, 0:48].rearrange("p (e b) -> p e b", b=2),
                                          in1=bT[:].unsqueeze(2).broadcast_to([128, 24, 2]), op=ALU.add), reads=[acc, bT], writes=[modT])
    S.op("dve", lambda e: e.scalar_tensor_tensor(out=modT[:, 8:16, :], in0=modT[:, 8:16, :], scalar=1.0,
                                                 in1=gT[:].unsqueeze(2).broadcast_to([128, 8, 2]), op0=ALU.add, op1=ALU.mult),
         reads=[modT, gT], writes=[modT])
    dst = [C.Sh, C.G, C.Gt]
    ident = C.k["ident"]
    ones = C.k["ones"]
    for b in range(2):
        for which in range(3):
            for half in range(2):
                pb = C.psum.next()
                for q in range(4):
                    ee = which * 8 + half * 4 + q
                    d = dg.next()
                    S.op("dve", lambda e, d=d, ee=ee, b=b: e.tensor_scalar(out=d[:], in0=ident[:], scalar1=modT[:, ee, b:b + 1], scalar2=None,
                                                                         op0=ALU.mult), reads=[ident, modT], writes=[d])
                    S.op("pe", lambda e, d=d, q=q, pb=pb: e.matmul(pb[:, q * 128:(q + 1) * 128], ones[:], d[:], start=True, stop=True),
                         reads=[ones, d], writes=[pb])
                tgt = dst[which][b]
                S.op("act", lambda e, tgt=tgt, half=half, pb=pb: e.copy(out=tgt[:, half * 512:(half + 1) * 512], in_=pb[:, :]),
                     reads=[pb], writes=[tgt], conc=(half > 0))
    S.release(mk)


def norm_tile(S, C, xt, b):
    sq = C.sqpool.next()
    st = C.stat.next()
    S.op("pool", lambda e: e.memset(st[:], 0.0), writes=[st])
    S.op("act", lambda e: e.activation(out=sq[:], in_=xt[:], func=AF.Square, accum_out=st[:, 0:1]),
         reads=[xt, st], writes=[sq, st])
    S.op("act", lambda e: e.activation(out=st[:, 1:2], in_=st[:, 0:1], func=AF.Sqrt, bias=EPS, scale=1.0 / D),
         reads=[st], writes=[st])
    S.op("dve", lambda e: e.reciprocal(out=st[:, 1:2], in_=st[:, 1:2]), reads=[st], writes=[st])
    h = C.hpool.next()
    S.op("dve", lambda e: e.scalar_tensor_tensor(out=h[:], in0=xt[:], scalar=st[:, 1:2], in1=C.G[b][:],
                                                 op0=ALU.mult, op1=ALU.mult), reads=[xt, st, C.G[b]], writes=[h])
    S.op("pool", lambda e: e.tensor_tensor(out=h[:], in0=h[:], in1=C.Sh[b][:], op=ALU.add), reads=[h, C.Sh[b]], writes=[h])
    return h


def transpose_to(S, C, dst, src_fn, n, width=128, rows=128, evac="act", pool=None):
    ident = C.k["ident"]
    j = 0
    first = True
    while j < n:
        m = min(4, n - j)
        pb = (pool or C.psum).next()
        for q in range(m):
            S.op("pe", lambda e, q=q, jj=j + q, pb=pb: e.transpose(pb[0:width, q * rows:(q + 1) * rows], src_fn(jj)[1],
                                                                 ident[0:rows, 0:rows]),
                 reads=[src_fn(j + q)[0], ident], writes=[pb])
        dv = dst[0:width, j:j + m, :]
        src = pb[0:width, 0:m * rows].rearrange("p (a b) -> p a b", b=rows)
        if evac == "act":
            S.op("act", lambda e, dv=dv, src=src: e.copy(out=dv, in_=src), reads=[pb], writes=[dst], conc=not first)
        else:
            S.op("dve", lambda e, dv=dv, src=src: e.tensor_copy(out=dv, in_=src), reads=[pb], writes=[dst], conc=not first)
        first = False
        j += m


def bc_reg(S, e):
    if getattr(S, "_bc_reg", None) is None:
        r = e.alloc_register("bc_rows")
        e.reg_mov(r, 4095)
        S._bc_reg = r
    return S._bc_reg


def moe_sublayer(S, C, ins, l, xin, xout, dbg=None):
    mod_tiles(S, C, ins, l, 1)
    k = C.k
    mk_moe = S.mark()
    if True:
        M = Ctx()
        if not hasattr(C, "moe_dram"):
            kd = "ExternalOutput" if DEBUG else "Internal"
            C.moe_dram = (S.dram("hbuf", [NTOK, D], kind=kd), S.dram("asg", [NBLK * 128, 3], I32, kind=kd),
                          S.dram("ybuf", [2 * NTOK + NBLK * 128, D], kind=kd))
        M.Wr = S.sb([128, 8, 20], F32, "Wr")
        M.LG = S.sb([128, NT, 20], F32, "LG")
        M.OH = S.sb([128, 2 * NT, 16], F32, "OH")
        M.Wt = S.sb([128, 2 * NT], F32, "Wt")
        M.t4 = [S.sb([128, NT, 4], F32, f"t4_{i}") for i in range(6)]
        M.t1 = [S.sb([128, NT], F32, f"t1_{i}") for i in range(8)]
        M.t16 = S.sb([128, NT, 16], F32, "t16")
        M.cnt = S.sb([128, 2 * NT, 16], F32, "cnt")
        M.pp = [S.sb([128, 2 * NT, 16], F32, f"pp{i}") for i in range(2)]
        M.e16 = [S.sb([128, 16], F32, f"e16_{i}") for i in range(6)]
        M.dest = S.sb([128, 2 * NT], F32, "dest")
        M.desti = S.sb([128, 2 * NT], I32, "desti")
        M.cmp = S.sb([128, NBLK, 16], F32, "cmp")
        M.blke = S.sb([128, NBLK], F32, "blke")
        M.idxw = S.sb([128, NBLK], I32, "idxw")
        M.info = S.sb([128, 2 * NT, 3], I32, "info")
        M.hbuf, M.asg, M.ybuf = C.moe_dram
        M.ab = Pool(S, 3, [128, 3], I32, "ab")
        M.xg = C.xpool
        M.xgT = C.hTpool
    S.load(M.Wr[:, :, 0:4], ins["moe_router_group"][l].rearrange("(k p) g -> p k g", p=128),
           reads=[ins["moe_router_group"]], writes=[M.Wr], allow_slow_non_contiguous=True)
    S.load(M.Wr[:, :, 4:20], ins["moe_router_expert"][l].rearrange("(k p) g -> p k g", p=128),
           reads=[ins["moe_router_expert"]], writes=[M.Wr], conc=True, allow_slow_non_contiguous=True)

    def p1_a(i):
        b = i // TPS
        xt = C.xpool.next()
        S.load(xt[:], xin.h[i * 128:(i + 1) * 128, :], reads=[xin.tiles[i]], writes=[xt])
        h = norm_tile(S, C, xt, b)
        S.load(M.hbuf.h[i * 128:(i + 1) * 128, :], h[:], reads=[h], writes=[M.hbuf], conc=(i > 0))
        return h

    def p1_b(i, h):
        hT = C.hTpool.next()
        transpose_to(S, C, hT, lambda j: (h, h[:, j * 128:(j + 1) * 128]), 8)
        pb = C.psum.next()
        for kk in range(8):
            S.op("pe", lambda e, kk=kk: e.matmul(pb[:, 0:20], hT[:, kk, :], M.Wr[:, kk, :], start=(kk == 0), stop=(kk == 7)),
                 reads=[hT, M.Wr], writes=[pb])
        S.op("act", lambda e: e.copy(out=M.LG[:, i, :], in_=pb[:, 0:20]), reads=[pb], writes=[M.LG], conc=(i > 0))

    hn = p1_a(0)
    for i in range(NT):
        hc_ = hn
        if i + 1 < NT:
            hn = p1_a(i + 1)
        p1_b(i, hc_)

    LG = M.LG
    lgg = LG[:, :, 0:4]
    mg, sg, m1, m2, dd, r1, w1, w2 = M.t1
    ohg, eg, el, oh1, el2, oh2 = M.t4
    V = lambda t: t[:]

    def bc4(t):
        return t[:].unsqueeze(2).broadcast_to([128, NT, 4])

    def dve(fn, reads, writes):
        S.op("dve", fn, reads=reads, writes=writes)

    dve(lambda e: e.tensor_reduce(out=mg[:], in_=lgg, axis=AX.X, op=ALU.max), [LG], [mg])
    dve(lambda e: e.tensor_tensor(out=ohg[:], in0=lgg, in1=bc4(mg), op=ALU.is_ge), [LG, mg], [ohg])
    dve(lambda e: e.tensor_tensor(out=eg[:], in0=lgg, in1=bc4(mg), op=ALU.subtract), [LG, mg], [eg])
    S.op("act", lambda e: e.activation(out=eg[:], in_=eg[:], func=AF.Exp), reads=[eg], writes=[eg])
    dve(lambda e: e.tensor_reduce(out=sg[:], in_=eg[:], axis=AX.X, op=ALU.add), [eg], [sg])
    dve(lambda e: e.reciprocal(out=sg[:], in_=sg[:]), [sg], [sg])
    dve(lambda e: e.tensor_tensor(out=M.t16[:].rearrange("p t (g j) -> p t g j", j=4),
                                  in0=LG[:, :, 4:20].rearrange("p t (g j) -> p t g j", j=4),
                                  in1=ohg[:].unsqueeze(3).broadcast_to([128, NT, 4, 4]), op=ALU.mult), [LG, ohg], [M.t16])
    dve(lambda e: e.tensor_reduce(out=el[:], in_=M.t16[:].rearrange("p t (g j) -> p t j g", j=4), axis=AX.X, op=ALU.add),
        [M.t16], [el])
    dve(lambda e: e.tensor_reduce(out=m1[:], in_=el[:], axis=AX.X, op=ALU.max), [el], [m1])
    dve(lambda e: e.tensor_tensor(out=oh1[:], in0=el[:], in1=bc4(m1), op=ALU.is_ge), [el, m1], [oh1])
    dve(lambda e: e.scalar_tensor_tensor(out=el2[:], in0=oh1[:], scalar=-1e30, in1=el[:], op0=ALU.mult, op1=ALU.add),
        [oh1, el], [el2])
    dve(lambda e: e.tensor_reduce(out=m2[:], in_=el2[:], axis=AX.X, op=ALU.max), [el2], [m2])
    dve(lambda e: e.tensor_tensor(out=oh2[:], in0=el2[:], in1=bc4(m2), op=ALU.is_ge), [el2, m2], [oh2])
    dve(lambda e: e.tensor_tensor(out=dd[:], in0=m2[:], in1=m1[:], op=ALU.subtract), [m1, m2], [dd])
    S.op("act", lambda e: e.activation(out=dd[:], in_=dd[:], func=AF.Exp), reads=[dd], writes=[dd])
    dve(lambda e: e.tensor_scalar(out=r1[:], in0=dd[:], scalar1=1.0, scalar2=None, op0=ALU.add), [dd], [r1])
    dve(lambda e: e.reciprocal(out=r1[:], in_=r1[:]), [r1], [r1])
    dve(lambda e: e.tensor_tensor(out=w1[:], in0=sg[:], in1=r1[:], op=ALU.mult), [sg, r1], [w1])
    dve(lambda e: e.tensor_tensor(out=w2[:], in0=sg[:], in1=w1[:], op=ALU.subtract), [sg, w1], [w2])
    OHv = M.OH[:].rearrange("p (t s) e -> p t s e", s=2)
    Wtv = M.Wt[:].rearrange("p (t s) -> p t s", s=2)
    for s_, oh, w in ((0, oh1, w1), (1, oh2, w2)):
        dve(lambda e, s_=s_, oh=oh: e.tensor_tensor(
            out=OHv[:, :, s_, :].rearrange("p t (g j) -> p t g j", j=4),
            in0=ohg[:].unsqueeze(3).broadcast_to([128, NT, 4, 4]),
            in1=oh[:].unsqueeze(2).broadcast_to([128, NT, 4, 4]), op=ALU.mult), [ohg, oh], [M.OH])
        dve(lambda e, s_=s_, w=w: e.tensor_copy(out=Wtv[:, :, s_], in_=w[:]), [w], [M.Wt])

    OHf = M.OH[:].rearrange("p s e -> p (s e)")
    rank = [C.psum.next() for _ in range(2)]
    cntp = [C.psum.next() for _ in range(2)]
    for hh in range(2):
        S.op("pe", lambda e, hh=hh: e.matmul(rank[hh][:, :], k["ustrict"][:], OHf[:, hh * 512:(hh + 1) * 512], start=True, stop=True),
             reads=[k["ustrict"], M.OH], writes=[rank[hh]])
        S.op("pe", lambda e, hh=hh: e.matmul(cntp[hh][:, :], k["ones"][:], OHf[:, hh * 512:(hh + 1) * 512], start=True, stop=True),
             reads=[k["ones"], M.OH], writes=[cntp[hh]])
    cntf = M.cnt[:].rearrange("p s e -> p (s e)")
    for hh in range(2):
        S.op("act", lambda e, hh=hh: e.copy(out=cntf[:, hh * 512:(hh + 1) * 512], in_=cntp[hh][:, :]),
             reads=[cntp[hh]], writes=[M.cnt], conc=(hh > 0))
    cur = M.cnt
    NS = 2 * NT
    sh = 1
    pi = 0
    while sh < NS:
        nxt = M.pp[pi]
        pi ^= 1
        S.op("pool", lambda e, cur=cur, nxt=nxt, sh=sh: e.tensor_copy(out=nxt[:, 0:sh, :], in_=cur[:, 0:sh, :]), reads=[cur], writes=[nxt])
        dve(lambda e, cur=cur, nxt=nxt, sh=sh: e.tensor_tensor(out=nxt[:, sh:NS, :], in0=cur[:, sh:NS, :], in1=cur[:, 0:NS - sh, :], op=ALU.add),
            [cur], [nxt])
        cur = nxt
        sh *= 2
    incl = cur
    tot, t127, md, padded, pe_a, pe_b = M.e16
    cmpv = M.cmp[:].rearrange("p j e -> p (j e)").rearrange("p (e j) -> p e j", j=NBLK)
    dve(lambda e: e.tensor_tensor(out=cmpv, in0=incl[:, NS - 1, :].unsqueeze(2).broadcast_to([128, 16, NBLK]),
                                  in1=k["thr"][:].unsqueeze(1).broadcast_to([128, 16, NBLK]), op=ALU.is_gt), [incl, k["thr"]], [M.cmp])
    dve(lambda e: e.tensor_reduce(out=md[:], in_=cmpv, axis=AX.X, op=ALU.add), [M.cmp], [md])
    dve(lambda e: e.tensor_scalar(out=padded[:], in0=md[:], scalar1=128.0, scalar2=None, op0=ALU.mult), [md], [padded])
    a, bb = padded, pe_a
    sh = 1
    while sh < 16:
        dst = pe_a if a is not pe_a else pe_b
        dve(lambda e, a=a, dst=dst, sh=sh: e.tensor_copy(out=dst[:, 0:sh], in_=a[:, 0:sh]), [a], [dst])
        dve(lambda e, a=a, dst=dst, sh=sh: e.tensor_tensor(out=dst[:, sh:16], in0=a[:, sh:16], in1=a[:, 0:16 - sh], op=ALU.add), [a], [dst])
        a = dst
        sh *= 2
    pad_end = a
    pad_start = tot
    dve(lambda e: e.tensor_tensor(out=pad_start[:], in0=pad_end[:], in1=padded[:], op=ALU.subtract), [pad_end, padded], [pad_start])
    basev = M.pp[pi]
    dve(lambda e: e.tensor_tensor(out=basev[:], in0=incl[:], in1=M.cnt[:], op=ALU.subtract), [incl, M.cnt], [basev])
    dve(lambda e: e.tensor_tensor(out=basev[:], in0=basev[:], in1=pad_start[:].unsqueeze(1).broadcast_to([128, NS, 16]), op=ALU.add),
        [basev, pad_start], [basev])
    basef = basev[:].rearrange("p s e -> p (s e)")
    for hh in range(2):
        dve(lambda e, hh=hh: e.tensor_tensor(out=basef[:, hh * 512:(hh + 1) * 512], in0=rank[hh][:, :], in1=basef[:, hh * 512:(hh + 1) * 512], op=ALU.add),
            [rank[hh], basev], [basev])
    dve(lambda e: e.tensor_tensor(out=basev[:], in0=basev[:], in1=M.OH[:], op=ALU.mult), [basev, M.OH], [basev])
    dve(lambda e: e.tensor_reduce(out=M.dest[:], in_=basev[:], axis=AX.X, op=ALU.add), [basev], [M.dest])
    dve(lambda e: e.tensor_copy(out=M.desti[:], in_=M.dest[:]), [M.dest], [M.desti])
    dve(lambda e: e.tensor_tensor(out=M.cmp[:], in0=pad_end[:].unsqueeze(1).broadcast_to([128, NBLK, 16]),
                                  in1=k["thr"][:].unsqueeze(2).broadcast_to([128, NBLK, 16]), op=ALU.is_le), [pad_end, k["thr"]], [M.cmp])
    dve(lambda e: e.tensor_reduce(out=M.blke[:], in_=M.cmp[:], axis=AX.X, op=ALU.add), [M.cmp], [M.blke])
    dve(lambda e: e.tensor_scalar(out=M.blke[:], in0=M.blke[:], scalar1=15.0, scalar2=128.0, op0=ALU.min, op1=ALU.mult), [M.blke], [M.blke])
    neq = M.cmp[:].rearrange("p j e -> p (j e)")[:, 0:NBLK]
    S.op("pool", lambda e: e.memset(neq[:, 0:2], 1.0), reads=[], writes=[M.cmp])
    dve(lambda e: e.tensor_tensor(out=neq[:, 2:NBLK], in0=M.blke[:, 2:NBLK], in1=M.blke[:, 0:NBLK - 2], op=ALU.not_equal), [M.blke, M.cmp], [M.cmp])
    dve(lambda e: e.tensor_scalar(out=M.blke[:], in0=M.blke[:], scalar1=k["piota"][:, 0:1], scalar2=float(l * 2048) - OOB_IDX, op0=ALU.add, op1=ALU.add),
        [M.blke, k["piota"]], [M.blke])
    dve(lambda e: e.tensor_tensor(out=M.blke[:], in0=M.blke[:], in1=neq, op=ALU.mult), [M.blke, M.cmp], [M.blke])
    dve(lambda e: e.tensor_scalar(out=M.blke[:], in0=M.blke[:], scalar1=OOB_IDX, scalar2=None, op0=ALU.add), [M.blke], [M.blke])
    dve(lambda e: e.tensor_copy(out=M.idxw[:], in_=M.blke[:]), [M.blke], [M.idxw])
    dve(lambda e: e.tensor_copy(out=M.info[:, :, 0], in_=k["tokid"][:]), [k["tokid"]], [M.info])
    dve(lambda e: e.tensor_copy(out=M.info[:, :, 1], in_=M.Wt[:].bitcast(I32)), [M.Wt], [M.info])
    dve(lambda e: e.tensor_copy(out=M.info[:, :, 2], in_=k["aid"][:]), [k["aid"]], [M.info])
    S.load(M.asg.h.ap().rearrange("(p j) c -> p j c", j=NBLK), k["init3"][:], reads=[k["init3"]], writes=[M.asg])
    for sg_ in range(NS):
        S.dma("pool", lambda e, sg_=sg_: e.indirect_dma_start(
            out=M.asg.h[:, :], out_offset=bass.IndirectOffsetOnAxis(ap=M.desti[:, sg_:sg_ + 1], axis=0),
            in_=M.info[:, sg_, :], in_offset=None), reads=[M.desti, M.info], writes=[M.asg], conc=True)

    dump(S, f"LG{l}", M.LG, [128, NT, 20]); dump(S, f"Wt{l}", M.Wt, [128, 2 * NT]); dump(S, f"OH{l}", M.OH, [128, 2 * NT, 16])
    dump(S, f"dest{l}", M.dest, [128, 2 * NT]); dump(S, f"blke{l}", M.blke, [128, NBLK]); dump(S, f"padend{l}", pad_end, [128, 16])
    for b_ in range(2):
        dump(S, f"G{l}{b_}", C.G[b_], [128, 1024]); dump(S, f"Sh{l}{b_}", C.Sh[b_], [128, 1024]); dump(S, f"Gt{l}{b_}", C.Gt[b_], [128, 1024])
    w1v = ins["moe_w1"].ap().rearrange("l e (p k) f -> (l e p) (k f)", k=8)
    w3v = ins["moe_w3"].ap().rearrange("l e (p k) f -> (l e p) (k f)", k=8)
    w2v = ins["moe_w2"].ap().rearrange("l e (p c) d -> (l e p) (c d)", c=4)
    mk_blk = S.mark()
    M.W1 = Pool(S, 2, [128, 4096], F32, "W1")
    M.W3 = Pool(S, 2, [128, 4096], F32, "W3")
    M.W2 = Pool(S, 2, [128, 4096], F32, "W2")
    M.g = Pool(S, 2, [128, 512], F32, "gg")
    M.gT = Pool(S, 2, [128, 4, 128], F32, "gT")
    M.y = Pool(S, 2, [128, 1024], F32, "yy")
    st = {}

    def stage_A(j):
        ab = M.ab.next()
        S.load(ab[:], M.asg.h[j * 128:(j + 1) * 128, :], reads=[M.asg], writes=[ab])
        xg = M.xg.next()
        S.dma("pool", lambda e, ab=ab, xg=xg: e.indirect_dma_start(
            out=xg[:], out_offset=None, in_=M.hbuf.h[:, :],
            in_offset=bass.IndirectOffsetOnAxis(ap=ab[:, 0:1], axis=0)), reads=[ab, M.hbuf], writes=[xg])
        Ws = []
        for wv, pool, nm in ((w1v, M.W1, "moe_w1"), (w3v, M.W3, "moe_w3"), (w2v, M.W2, "moe_w2")):
            W = pool.next()
            S.dma("pool", lambda e, W=W, wv=wv, j=j: e.indirect_dma_start(
                out=W[:], out_offset=None, in_=wv,
                in_offset=bass.IndirectOffsetOnAxis(ap=M.idxw[:, j:j + 1], axis=0),
                bounds_check=bc_reg(S, e), oob_is_err=False), reads=[M.idxw, ins[nm]], writes=[W])
            Ws.append(W)
        xgT = M.xgT.next()
        transpose_to(S, C, xgT, lambda kk, xg=xg: (xg, xg[:].rearrange("t (p k) -> t k p", k=8)[:, kk, :]), 8)
        st[j] = dict(ab=ab, W=Ws, xgT=xgT)

    def stage_B(j):
        d = st[j]
        xgT = d["xgT"]
        W1, W3, W2 = d["W"]
        p1, p3 = C.psum.next(), C.psum.next()
        for kk in range(8):
            S.op("pe", lambda e, kk=kk, xgT=xgT, W1=W1, p1=p1: e.matmul(p1[:, :], xgT[:, kk, :], W1[:, kk * 512:(kk + 1) * 512],
                                                                  start=(kk == 0), stop=(kk == 7)), reads=[xgT, W1], writes=[p1])
        for kk in range(8):
            S.op("pe", lambda e, kk=kk, xgT=xgT, W3=W3, p3=p3: e.matmul(p3[:, :], xgT[:, kk, :], W3[:, kk * 512:(kk + 1) * 512],
                                                                  start=(kk == 0), stop=(kk == 7)), reads=[xgT, W3], writes=[p3])
        g = M.g.next()
        S.op("act", lambda e, g=g, p1=p1: e.activation(out=g[:], in_=p1[:, :], func=AF.Silu), reads=[p1], writes=[g])
        S.op("dve", lambda e, g=g, p3=p3: e.tensor_tensor(out=g[:], in0=g[:], in1=p3[:, :], op=ALU.mult), reads=[g, p3], writes=[g])
        d["g"] = g

    def stage_C(j):
        d = st[j]
        g = d["g"]
        gT = M.gT.next()
        transpose_to(S, C, gT, lambda cc, g=g: (g, g[:].rearrange("t (p c) -> t c p", c=4)[:, cc, :]), 4)
        d["gT"] = gT

    def stage_D(j):
        d = st.pop(j)
        gT, ab = d["gT"], d["ab"]
        W2 = d["W"][2]
        y = M.y.next()
        for hh in range(2):
            py = C.psum.next()
            for cc in range(4):
                S.op("pe", lambda e, cc=cc, hh=hh, gT=gT, W2=W2, py=py: e.matmul(
                    py[:, :], gT[:, cc, :], W2[:, cc * 1024 + hh * 512: cc * 1024 + (hh + 1) * 512],
                    start=(cc == 0), stop=(cc == 3)), reads=[gT, W2], writes=[py])
            S.op("act" if hh == 0 else "dve",
                 (lambda e, hh=hh, y=y, py=py, ab=ab: e.activation(out=y[:, 0:512], in_=py[:, :], func=AF.Identity, scale=ab[:, 1:2].bitcast(F32)))
                 if hh == 0 else
                 (lambda e, hh=hh, y=y, py=py, ab=ab: e.tensor_scalar(out=y[:, 512:1024], in0=py[:, :], scalar1=ab[:, 1:2].bitcast(F32),
                                                                   scalar2=None, op0=ALU.mult)),
                 reads=[py, ab], writes=[y], conc=(hh > 0))
        S.dma("pool", lambda e, ab=ab, y=y: e.indirect_dma_start(
            out=M.ybuf.h[:, :], out_offset=bass.IndirectOffsetOnAxis(ap=ab[:, 2:3], axis=0),
            in_=y[:], in_offset=None), reads=[ab, y], writes=[M.ybuf], conc=(j > 0))

    stage_A(0)
    for j in range(NBLK):
        stage_B(j)
        if j + 1 < NBLK:
            stage_A(j + 1)
        stage_C(j)
        stage_D(j)

    S.release(mk_blk)
    M.yy = Pool(S, 2, [128, 2, 1024], F32, "ycomb")
    for i in range(NT):
        b = i // TPS
        yy = M.yy.next()
        S.load(yy[:], M.ybuf.h[i * 256:(i + 1) * 256, :].rearrange("(t s) d -> t s d", s=2), reads=[M.ybuf], writes=[yy])
        xt = C.xpool.next()
        S.load(xt[:], xin.h[i * 128:(i + 1) * 128, :], reads=[xin.tiles[i]], writes=[xt])
        S.op("pool", lambda e, yy=yy: e.tensor_tensor(out=yy[:, 0, :], in0=yy[:, 0, :], in1=yy[:, 1, :], op=ALU.add), reads=[yy], writes=[yy])
        S.op("dve", lambda e, yy=yy, b=b: e.tensor_tensor(out=yy[:, 0, :], in0=yy[:, 0, :], in1=C.Gt[b][:], op=ALU.mult), reads=[yy, C.Gt[b]], writes=[yy])
        S.op("pool", lambda e, yy=yy, xt=xt: e.tensor_tensor(out=xt[:], in0=xt[:], in1=yy[:, 0, :], op=ALU.add), reads=[yy, xt], writes=[xt])
        S.load(xout.h[i * 128:(i + 1) * 128, :], xt[:], reads=[xt], writes=[xout.tiles[i]])
    S.release(mk_moe)


def TV(ap, res):
    return T(ap, res=res)


def outproj_phase(S, C, ins, w_out, obuf, xin, xout):
    mk = S.mark()
    Wo = S.sb([128, 8, 1024], F32, "Wo")
    S.load(Wo[:], w_out.rearrange("(k p) n -> p k n", p=128), reads=[], writes=[Wo])
    opool = Pool(S, 2, [128, 1024], F32, "ot")

    def stage_a(i):
        ot = opool.next()
        S.load(ot[:], obuf.h[i * 128:(i + 1) * 128, :], reads=[obuf], writes=[ot])
        oT = C.hTpool.next()
        transpose_to(S, C, oT, lambda jj: (ot, ot[:, jj * 128:(jj + 1) * 128]), 8)
        xt = C.xpool.next()
        S.load(xt[:], xin.h[i * 128:(i + 1) * 128, :], reads=[xin.tiles[i]], writes=[xt])
        return ot, oT, xt

    def stage_b(i, ot, oT, xt):
        b = i // TPS
        for hh in range(2):
            py = C.psum.next()
            for kk in range(8):
                S.op("pe", lambda e, kk=kk, hh=hh, py=py: e.matmul(py[:, :], oT[:, kk, :], Wo[:, kk, hh * 512:(hh + 1) * 512],
                                                                start=(kk == 0), stop=(kk == 7)), reads=[oT, Wo], writes=[py])
            S.op("dve", lambda e, hh=hh, py=py: e.tensor_tensor(out=ot[:, hh * 512:(hh + 1) * 512], in0=py[:, :],
                                                                in1=C.Gt[b][:, hh * 512:(hh + 1) * 512], op=ALU.mult),
                 reads=[py, C.Gt[b], oT], writes=[ot])
        S.op("pool", lambda e: e.tensor_tensor(out=xt[:], in0=xt[:], in1=ot[:], op=ALU.add), reads=[ot, xt], writes=[xt])
        S.load(xout.h[i * 128:(i + 1) * 128, :], xt[:], reads=[xt], writes=[xout.tiles[i]])

    nxt = stage_a(0)
    for i in range(NT):
        cur_ = nxt
        if i + 1 < NT:
            nxt = stage_a(i + 1)
        stage_b(i, *cur_)
    S.release(mk)


def hgrn_sublayer(S, C, ins, l, xin, xout):
    j = l // 2
    mod_tiles(S, C, ins, l, 0)
    k = C.k
    mk = S.mark()
    GT = 4
    if not hasattr(C, "obuf"):
        C.obuf = S.dram("obuf", [NTOK, D])
    obuf = C.obuf
    lbB = S.sb([128, 1024], F32, "lbB")
    omlB = S.sb([128, 1024], F32, "omlB")
    gainB = S.sb([128, 128], F32, "gainB")
    lbd = ins["hgrn_lower_bounds"]
    S.load(lbB[:], lbd[l:l + 1, :].broadcast_to([128, 1024]), reads=[lbd], writes=[lbB])
    S.load(omlB[:], lbd[0:1, :].broadcast_to([128, 1024]), reads=[lbd], writes=[omlB])
    S.load(gainB[:], ins["hgrn_out_gain"][j:j + 1, :].broadcast_to([128, 128]), reads=[ins["hgrn_out_gain"]], writes=[gainB])
    S.op("dve", lambda e: e.tensor_tensor(out=lbB[:], in0=lbB[:], in1=omlB[:], op=ALU.subtract), reads=[lbB, omlB], writes=[lbB])
    S.op("act", lambda e: e.activation(out=lbB[:], in_=lbB[:], func=AF.Sigmoid), reads=[lbB], writes=[lbB])
    S.op("dve", lambda e: e.tensor_scalar(out=omlB[:], in0=lbB[:], scalar1=-1.0, scalar2=1.0, op0=ALU.mult, op1=ALU.add), reads=[lbB], writes=[omlB])
    hTgp = Pool(S, 1, [128, 8, GT * 128], F32, "hTg")
    Wp = Pool(S, 3, [128, 8, 512], F32, "Wh")
    NBUF = 2
    HB = []
    for i_ in range(NBUF):
        d = {}
        d["z"] = S.sb([128, GT, 512], F32, f"z{i_}")
        for n in ("fb", "kb", "lf", "qs", "e1", "qt", "kt", "kh", "oall", "sqb"):
            d[n] = S.sb([128, GT, 128], F32, f"{n}{i_}")
        d["av"] = S.sb([128, GT * 4], F32, f"av{i_}")
        d["ss"] = S.sb([128, GT], F32, f"ss{i_}")
        HB.append(d)
    Sst = S.sb([128, 8, 128], F32, "Sst")
    Sres = [Res(f"S{h}") for h in range(8)]
    Spool = Pool(S, 9, [128, 128], F32, "Sp")
    qkTp = Pool(S, 2, [128, 256], F32, "qkT")
    Zp = Pool(S, 2, [128, 4, 128], F32, "Z")
    Ap = Pool(S, 2, [128, 128], F32, "A")
    Vp = Pool(S, 2, [128, 4, 128], F32, "Vb")
    w_in = ins["hgrn_w_in"]

    def bcT(t, h):
        return t[:, h * 128:(h + 1) * 128].unsqueeze(1).broadcast_to([128, GT, 128])

    ps_front = Pool(S, 4, None, tiles=C.psum.t[0:4])
    ps_o = Pool(S, 2, None, tiles=C.psum.t[4:6])
    ps_prep = Pool(S, 2, None, tiles=C.psum.t[6:8])

    def prep_group(b, grp):
        hTg = hTgp.next()
        for tt in range(GT):
            i = b * TPS + grp * GT + tt
            xt = C.xpool.next()
            S.load(xt[:], xin.h[i * 128:(i + 1) * 128, :], reads=[xin.tiles[i]], writes=[xt])
            h_ = norm_tile(S, C, xt, b)
            transpose_to(S, C, TV(hTg[:, :, tt * 128:(tt + 1) * 128], hTg.res),
                         lambda jj, h_=h_: (h_, h_[:, jj * 128:(jj + 1) * 128]), 8, pool=ps_prep)
        return hTg

    def load_W(h):
        W = Wp.next()
        Wres = [Res(f"W{q}") for q in range(4)]
        for q in range(4):
            S.load(W[:, :, q * 128:(q + 1) * 128],
                   w_in[j, :, q * 1024 + h * 128: q * 1024 + (h + 1) * 128].rearrange("(k p) n -> p k n", p=128),
                   reads=[w_in], writes=[W], conc=(q > 0))
        return W

    def prep_head(hTg, h, B, W):
        z, fb, kb, lf, qs, e1, qt, kt, kh, av = (B[n] for n in ("z", "fb", "kb", "lf", "qs", "e1", "qt", "kt", "kh", "av"))
        for tt in range(GT):
            pz = ps_prep.next()
            for kk in range(8):
                S.op("pe", lambda e, kk=kk, tt=tt, W=W, pz=pz: e.matmul(pz[:, :], hTg[:, kk, tt * 128:(tt + 1) * 128], W[:, kk, :],
                                                                      start=(kk == 0), stop=(kk == 7)), reads=[hTg, W], writes=[pz])
            S.op("act", lambda e, tt=tt, pz=pz: e.copy(out=z[:, tt, :], in_=pz[:, :]), reads=[pz], writes=[z], conc=(tt > 0))
        zq, zf, zi, zg = (z[:, :, q * 128:(q + 1) * 128] for q in range(4))
        S.op("act", lambda e: e.activation(out=fb[:], in_=zf, func=AF.Sigmoid), reads=[z], writes=[fb])
        S.op("act", lambda e: e.activation(out=qs[:], in_=zq, func=AF.Silu), reads=[z], writes=[qs])
        S.op("dve", lambda e: e.tensor_tensor(out=fb[:], in0=fb[:], in1=bcT(omlB, h), op=ALU.mult), reads=[fb, omlB], writes=[fb])
        S.op("dve", lambda e: e.tensor_tensor(out=fb[:], in0=fb[:], in1=bcT(lbB, h), op=ALU.add), reads=[fb, lbB], writes=[fb])
        S.op("dve", lambda e: e.tensor_scalar(out=kb[:], in0=fb[:], scalar1=-1.0, scalar2=1.0, op0=ALU.mult, op1=ALU.add), reads=[fb], writes=[kb])
        S.op("act", lambda e: e.activation(out=lf[:], in_=fb[:], func=AF.Ln), reads=[fb], writes=[lf])
        return lambda: prep_head2(B)

    def prep_head2(B):
        z, fb, kb, lf, qs, e1, qt, kt, kh, av = (B[n] for n in ("z", "fb", "kb", "lf", "qs", "e1", "qt", "kt", "kh", "av"))
        lff = lf[:].rearrange("p t d -> p (t d)")
        pc, pr = ps_prep.next(), ps_prep.next()
        S.op("pe", lambda e: e.matmul(pc[:, :], k["ltri"][:], lff, start=True, stop=True), reads=[k["ltri"], lf], writes=[pc])
        S.op("pe", lambda e: e.matmul(pr[:, :], k["urev"][:], lff, start=True, stop=True), reads=[k["urev"], lf], writes=[pr])
        e1f = e1[:].rearrange("p t d -> p (t d)")
        S.op("act", lambda e: e.activation(out=e1f, in_=pc[:, :], func=AF.Exp), reads=[pc], writes=[e1])
        S.op("dve", lambda e: e.tensor_tensor(out=qt[:], in0=qs[:], in1=e1[:], op=ALU.mult), reads=[qs, e1], writes=[qt])
        S.op("act", lambda e: e.activation(out=e1f, in_=pc[:, :], func=AF.Exp, scale=-1.0), reads=[pc, qt], writes=[e1])
        S.op("dve", lambda e: e.tensor_tensor(out=kt[:], in0=kb[:], in1=e1[:], op=ALU.mult), reads=[kb, e1], writes=[kt])
        S.op("act", lambda e: e.activation(out=e1f, in_=pr[:, :], func=AF.Exp), reads=[pr, kt], writes=[e1])
        S.op("dve", lambda e: e.tensor_tensor(out=kh[:], in0=kb[:], in1=e1[:], op=ALU.mult), reads=[kb, e1], writes=[kh])
        pl = ps_prep.next()
        for tt in range(GT):
            S.op("pe", lambda e, tt=tt: e.matmul(pl[:, tt * 4:(tt + 1) * 4], lf[:, tt, :], k["cm"][:], start=True, stop=True),
                 reads=[lf, k["cm"]], writes=[pl])
        S.op("act", lambda e: e.activation(out=av[:], in_=pl[:, 0:GT * 4], func=AF.Exp), reads=[pl], writes=[av])

    def tiles_head(b, grp, h, B, mid_hook=None):
        z, qt, kt, kh, av, oall, sqb, ss = (B[n] for n in ("z", "qt", "kt", "kh", "av", "oall", "sqb", "ss"))
        zi, zg = z[:, :, 256:384], z[:, :, 384:512]
        cur = {"S": TV(Sst[:, h, :], Sres[h])}

        def front(tt):
            pt = ps_front.next()
            S.op("pe", lambda e: e.transpose(pt[:, 0:128], qt[:, tt, :], k["ident"][:]), reads=[qt, k["ident"]], writes=[pt])
            S.op("pe", lambda e: e.transpose(pt[:, 128:256], kt[:, tt, :], k["ident"][:]), reads=[kt, k["ident"]], writes=[pt])
            qkT = qkTp.next()
            S.op("act", lambda e: e.copy(out=qkT[:], in_=pt[:, 0:256]), reads=[pt], writes=[qkT])
            Z = Zp.next()
            S.op("dve", lambda e: e.tensor_tensor(out=Z[:], in0=pt[:, 0:128].unsqueeze(1).broadcast_to([128, 4, 128]),
                                                  in1=k["cmrow"][:].rearrange("p (c t) -> p c t", t=128), op=ALU.mult),
                 reads=[pt, k["cmrow"]], writes=[Z])
            pa = pt
            S.op("pe", lambda e: e.matmul(pa[:, 256:384], qkT[:, 128:256], qkT[:, 0:128], start=True, stop=True), reads=[qkT], writes=[pa])
            A = Ap.next()
            S.op("dve", lambda e: e.tensor_tensor(out=A[:], in0=pa[:, 256:384], in1=k["ltri"][:], op=ALU.mult), reads=[pa, k["ltri"]], writes=[A])
            Vb = Vp.next()
            S.op("pool", lambda e: e.tensor_tensor(out=Vb[:], in0=zi[:, tt, :].unsqueeze(1).broadcast_to([128, 4, 128]),
                                                   in1=k["cm"][:].unsqueeze(2).broadcast_to([128, 4, 128]), op=ALU.mult),
                 reads=[z, k["cm"]], writes=[Vb])
            pkv = ps_front.next()
            S.op("pe", lambda e: e.matmul(pkv[:, :], kh[:, tt, :], Vb[:].rearrange("p c e -> p (c e)"), start=True, stop=True),
                 reads=[kh, Vb], writes=[pkv])
            Ss = []
            for c in range(4):
                Scur = cur["S"]
                Ss.append(Scur)
                Sn = Spool.next() if not (tt == GT - 1 and c == 3) else TV(Sst[:, h, :], Sres[h])
                S.op("dve", lambda e, c=c, Sn=Sn, Scur=Scur: e.scalar_tensor_tensor(
                    out=Sn[:], in0=Scur[:], scalar=av[:, tt * 4 + c:tt * 4 + c + 1], in1=pkv[:, c * 128:(c + 1) * 128],
                    op0=ALU.mult, op1=ALU.add), reads=[Scur, av, pkv], writes=[Sn])
                cur["S"] = Sn
            return dict(Z=Z, A=A, Ss=Ss)

        def back(tt, d):
            Z, A, Ss = d["Z"], d["A"], d["Ss"]
            po = ps_o.next()
            S.op("pe", lambda e: e.matmul(po[:, 0:128], A[:], zi[:, tt, :], start=True, stop=False), reads=[A, z], writes=[po])
            for c in range(4):
                Scur = Ss[c]
                S.op("pe", lambda e, c=c, Scur=Scur: e.matmul(po[:, 0:128], Z[:, c, :], Scur[:], start=False, stop=(c == 3)),
                     reads=[Z, Scur], writes=[po])
            S.op("act", lambda e: e.copy(out=oall[:, tt, :], in_=po[:, 0:128]), reads=[po], writes=[oall], conc=(tt > 0))

        d = front(0)
        for tt in range(GT):
            dn = front(tt + 1) if tt + 1 < GT else None
            back(tt, d)
            d = dn
            if tt == 0 and mid_hook is not None:
                mid_hook()
        S.op("act", lambda e: e.activation(out=sqb[:], in_=oall[:], func=AF.Square), reads=[oall], writes=[sqb])
        S.op("dve", lambda e: e.tensor_reduce(out=ss[:], in_=sqb[:], axis=AX.X, op=ALU.add), reads=[sqb], writes=[ss])
        S.op("act", lambda e: e.activation(out=ss[:], in_=ss[:], func=AF.Sqrt, bias=EPS, scale=1.0 / 128), reads=[ss], writes=[ss])
        S.op("dve", lambda e: e.reciprocal(out=ss[:], in_=ss[:]), reads=[ss], writes=[ss])
        S.op("dve", lambda e: e.tensor_tensor(out=oall[:], in0=oall[:], in1=ss[:].unsqueeze(2).broadcast_to([128, GT, 128]), op=ALU.mult),
             reads=[oall, ss], writes=[oall])
        S.op("dve", lambda e: e.tensor_tensor(out=oall[:], in0=oall[:], in1=gainB[:].unsqueeze(1).broadcast_to([128, GT, 128]), op=ALU.mult),
             reads=[oall, gainB], writes=[oall])
        S.op("act", lambda e: e.activation(out=sqb[:], in_=zg, func=AF.Silu), reads=[z], writes=[sqb])
        S.op("dve", lambda e: e.tensor_tensor(out=oall[:], in0=oall[:], in1=sqb[:], op=ALU.mult), reads=[oall, sqb], writes=[oall])
        r0 = (b * TPS + grp * GT) * 128
        S.load(obuf.h[r0:r0 + GT * 128, h * 128:(h + 1) * 128].rearrange("(t p) e -> p t e", p=128), oall[:],
               reads=[oall], writes=[obuf], conc=not (b == 0 and grp == 0 and h == 0))

    work = [(b, grp, h) for b in range(NB) for grp in range(TPS // GT) for h in range(8)]
    hT_of = {}
    nb = 0

    Wq = {}

    def do_loadW(idx):
        if idx < len(work) and idx not in Wq:
            Wq[idx] = load_W(work[idx][2])

    def do_prep(idx):
        b, grp, h = work[idx]
        if h == 0:
            hT_of[(b, grp)] = prep_group(b, grp)
        return prep_head(hT_of[(b, grp)], h, HB[idx % NBUF], Wq.pop(idx))

    do_loadW(0)
    do_loadW(1)
    p2 = do_prep(0)
    p2()
    for idx, (b, grp, h) in enumerate(work):
        if grp == 0 and h == 0:
            S.op("pool", lambda e: e.memset(Sst[:], 0.0), writes=[Sst] + Sres)
        do_loadW(idx + 2)
        hook = do_prep(idx + 1) if idx + 1 < len(work) else None
        tiles_head(b, grp, h, HB[idx % NBUF], hook)
    S.release(mk)
    outproj_phase(S, C, ins, ins["hgrn_w_out"][j], obuf, xin, xout)


def dram_pitch(t, offset, pitch, rows, n):
    return AP(t.h.ap().tensor, offset, [[pitch, rows], [1, n]])


def nsa_sublayer(S, C, ins, l, xin, xout):
    j = l // 2
    mod_tiles(S, C, ins, l, 0)
    k = C.k
    ident = k["ident"]
    if not hasattr(C, "obuf"):
        C.obuf = S.dram("obuf", [NTOK, D])
    obuf = C.obuf
    hTbuf = S.dram("hTbuf", [NT, 128, 8, 128])
    LS, LW, LC = 2560, 1536, 4096
    TS = S.dram("tabS", [16, 128 * (LS + 1)])
    TW = S.dram("tabW", [16, 128 * (LW + 1)])
    TC = S.dram("tabC", [16, 128 * (LC + 16) + LC])
    mk_phase = S.mark()

    def n1_a(i):
        xt = C.xpool.next()
        S.load(xt[:], xin.h[i * 128:(i + 1) * 128, :], reads=[xin.tiles[i]], writes=[xt])
        return norm_tile(S, C, xt, i // TPS)

    def n1_b(i, h_):
        hT = C.hTpool.next()
        transpose_to(S, C, hT, lambda jj: (h_, h_[:, jj * 128:(jj + 1) * 128]), 8)
        S.load(hTbuf.h[i], hT[:], reads=[hT], writes=[hTbuf], conc=(i > 0))

    hn = n1_a(0)
    for i in range(NT):
        hc_ = hn
        if i + 1 < NT:
            hn = n1_a(i + 1)
        n1_b(i, hc_)

    mk_tab = S.mark()
    oh = S.sb([32, 2048], F32, "bkoh")
    rb = S.sb([32, 16], F32, "rb")
    BBs = [S.sb([128, LC + 16], F32, f"BB{i}") for i in range(2)]
    BWs = [S.sb([128, LW + 1], F32, f"BW{i}") for i in range(2)]
    rbbp = Pool(S, 2, [32, 128], F32, "rbb")
    S.load(oh[:], ins["bkoh"].ap(), reads=[ins["bkoh"]], writes=[oh])
    S.load(rb[:], ins["rel_bias"].ap(), reads=[ins["rel_bias"]], writes=[rb])
    for i_ in range(2):
        S.op("pool", lambda e, i_=i_: e.memset(BBs[i_][:], NEG), writes=[BBs[i_]])
        S.op("pool", lambda e, i_=i_: e.memset(BWs[i_][:], NEG), writes=[BWs[i_]])
    for h in range(16):
        BB, BW = BBs[h % 2], BWs[h % 2]
        rbb = rbbp.next()
        S.op("dve", lambda e, h=h, rbb=rbb: e.tensor_copy(out=rbb[:], in_=rb[:, h:h + 1].broadcast_to([32, 128])), reads=[rb], writes=[rbb])
        for c in range(4):
            pb = C.psum.next()
            S.op("pe", lambda e, c=c, rbb=rbb, pb=pb: e.matmul(pb[:, :], rbb[:], oh[:, c * 512:(c + 1) * 512], start=True, stop=True),
                 reads=[rbb, oh], writes=[pb])
            S.op("act", lambda e, c=c, pb=pb, BB=BB: e.copy(out=BB[:, 2048 + c * 512:2048 + (c + 1) * 512], in_=pb[:, :]),
                 reads=[pb], writes=[BB], conc=(c > 0))
        S.op("dve", lambda e, BB=BB, BW=BW: e.tensor_copy(out=BW[:, 0:1024], in_=BB[:, 1536:2560]), reads=[BB], writes=[BW])
        S.load(dram_pitch(TS, h * 128 * (LS + 1), LS + 1, 128, LS + 1), BB[:, 1536:1536 + LS + 1], reads=[BB], writes=[TS], conc=True)
        S.load(dram_pitch(TW, h * 128 * (LW + 1), LW + 1, 128, LW + 1), BW[:], reads=[BW], writes=[TW], conc=True)
        S.load(dram_pitch(TC, h * (128 * (LC + 16) + LC), LC + 16, 128, LC + 16), BB[:, :], reads=[BB], writes=[TC], conc=True)
    S.release(mk_tab)

    cmnf = S.sb([128, TPS, 32], F32, "cmnf")
    sadd = S.sb([128, TPS, 32], F32, "sadd")
    for nm, t_ in (("cmnf", cmnf), ("sadd", sadd)):
        S.load(t_[:], ins[nm].ap(), reads=[ins[nm]], writes=[t_])
    gain8 = S.sb([128, 8, 64], F32, "gain8")
    kg0B = S.sb([128, 64], F32, "kg0B")
    for q in range(4):
        S.load(gain8[:, q, :], ins["nsa_q_gain"][j:j + 1, :].broadcast_to([128, 64]), reads=[ins["nsa_q_gain"]], writes=[gain8], conc=(q > 0))
    for q in range(2):
        S.load(gain8[:, 4 + q, :], ins["nsa_k_gain"][j, 1 + q:2 + q, :].broadcast_to([128, 64]), reads=[ins["nsa_k_gain"]],
               writes=[gain8], conc=True)
    S.load(kg0B[:], ins["nsa_k_gain"][j, 0:1, :].broadcast_to([128, 64]), reads=[ins["nsa_k_gain"]], writes=[kg0B])
    S.op("dve", lambda e: e.tensor_scalar(out=gain8[:, 0:4, :], in0=gain8[:, 0:4, :], scalar1=0.125, scalar2=None, op0=ALU.mult),
         reads=[gain8], writes=[gain8])
    W1c = S.sb([128, 32, 64], F32, "W1c")
    peT = S.sb([128, 32], F32, "peT")
    W2c = S.sb([64, 2, 64], F32, "W2c")
    CST = S.sb([64, 2], F32, "CST")
    for a in range(2):
        S.load(W1c[a * 64:(a + 1) * 64, :, :], ins["nsa_cmp_w1"][j, a].rearrange("(l d) n -> d l n", d=64), reads=[ins["nsa_cmp_w1"]],
               writes=[W1c], conc=(a > 0))
        S.load(peT[a * 64:(a + 1) * 64, :], ins["nsa_cmp_pe"][j, a].rearrange("l d -> d l"), reads=[ins["nsa_cmp_pe"]], writes=[peT],
               conc=(a > 0), allow_slow_non_contiguous=True)
        S.load(W2c[:, a, :], ins["nsa_cmp_w2"][j, a], reads=[ins["nsa_cmp_w2"]], writes=[W2c], conc=(a > 0))
    for a in range(2):
        pb = C.psum.next()
        for l_ in range(32):
            S.op("pe", lambda e, a=a, l_=l_, pb=pb: e.matmul(pb[0:64, 0:1], W1c[a * 64:(a + 1) * 64, l_, :], peT[a * 64:(a + 1) * 64, l_:l_ + 1],
                                                          start=(l_ == 0), stop=(l_ == 31)), reads=[W1c, peT], writes=[pb])
        S.op("act", lambda e, a=a, pb=pb: e.copy(out=CST[:, a:a + 1], in_=pb[0:64, 0:1]), reads=[pb], writes=[CST], conc=(a > 0))
    w_in = ins["nsa_w_in"]
    exv = ins["ex"].ap().rearrange("s t k -> s (t k)")

    for b in range(NB):
        for g in range(4):
            mk_g = S.mark()
            ALLT = S.sb([128, 6, T_SEQ], F32, "ALLT")
            VA = S.sb([128, TPS, 2, 65], F32, "VA")
            GA = S.sb([128, TPS, 12], F32, "GA")
            KCN = S.sb([128, 127], F32, "KCN")
            VCA = S.sb([128, 97], F32, "VCA")
            oacc = S.sb([128, TPS, 4, 64], F32, "oacc")
            IMP = S.sb([128, TPS, 32], F32, "IMP")
            S.op("pool", lambda e, ALLT=ALLT: e.memset(ALLT[64:128, :, :], 0.0), writes=[ALLT])
            S.load(ALLT[64:96, 4, :], exv, reads=[ins["ex"]], writes=[ALLT], conc=True)
            S.op("pool", lambda e, KCN=KCN: e.memset(KCN[:], 0.0), writes=[KCN])
            S.op("pool", lambda e, VA=VA: e.memset(VA[:, :, :, 64:65], 1.0), writes=[VA])
            S.op("pool", lambda e, VCA=VCA: e.memset(VCA[:, 64:65], 1.0), writes=[VCA])
            S.load(VCA[0:127, 65:97], ins["c2s"].ap(), reads=[ins["c2s"]], writes=[VCA], conc=True)
            S.op("pool", lambda e, oacc=oacc: e.memset(oacc[:], 0.0), writes=[oacc])
            S.op("pool", lambda e, IMP=IMP: e.memset(IMP[:], 0.0), writes=[IMP])
            mk_p = S.mark()
            RAW = S.sb([128, 1, T_SEQ], F32, "RAW")
            Wg = S.sb([128, 8, 652], F32, "Wg")
            stgp = Pool(S, 2, [128, 8, 64], F32, "stg")
            sqn = S.sb([128, 384], F32, "sqn")
            st8p = Pool(S, 2, [128, 6], F32, "st6")
            cols = [(g * 256, 256)] + [(c0 + g * 64, 64) for c0 in (1536, 2048, 1024, 1280, 1792, 2304)] + [(2560 + 12 * g, 12)]
            off = 0
            for ci, (c0, wd) in enumerate(cols):
                S.load(Wg[:, :, off:off + wd], w_in[j, :, c0:c0 + wd].rearrange("(k p) n -> p k n", p=128),
                       reads=[w_in], writes=[Wg], conc=(ci > 0))
                off += wd
            ps_mm = Pool(S, 4, None, tiles=C.psum.t[0:4])
            ps_tr = Pool(S, 4, None, tiles=C.psum.t[4:8])

            def proj_mm(t):
                i = b * TPS + t
                hT = C.hTpool.next()
                S.load(hT[:], hTbuf.h[i], reads=[hTbuf], writes=[hT])
                pA, pB = ps_mm.next(), ps_mm.next()
                for kk in range(8):
                    S.op("pe", lambda e, kk=kk: e.matmul(pA[:, :], hT[:, kk, :], Wg[:, kk, 0:512], start=(kk == 0), stop=(kk == 7)),
                         reads=[hT, Wg], writes=[pA])
                for kk in range(8):
                    S.op("pe", lambda e, kk=kk: e.matmul(pB[:, 0:140], hT[:, kk, :], Wg[:, kk, 512:652], start=(kk == 0), stop=(kk == 7)),
                         reads=[hT, Wg], writes=[pB])
                return pA, pB

            def proj_post(t, pA, pB):
                st8 = st8p.next()
                stg = stgp.next()
                S.op("act", lambda e: e.activation(out=sqn[:], in_=pA[:, 0:384], func=AF.Square), reads=[pA], writes=[sqn])
                S.op("dve", lambda e: e.tensor_reduce(out=st8[:], in_=sqn[:].rearrange("p (a d) -> p a d", d=64), axis=AX.X, op=ALU.add),
                     reads=[sqn], writes=[st8])
                S.op("act", lambda e: e.activation(out=st8[:], in_=st8[:], func=AF.Sqrt, bias=EPS, scale=1.0 / 64), reads=[st8], writes=[st8])
                S.op("dve", lambda e: e.reciprocal(out=st8[:], in_=st8[:]), reads=[st8], writes=[st8])
                S.op("dve", lambda e: e.tensor_tensor(
                    out=stg[:, 0:6, :], in0=pA[:, 0:384].rearrange("p (a d) -> p a d", d=64), in1=st8[:].unsqueeze(2).broadcast_to([128, 6, 64]), op=ALU.mult),
                    reads=[pA, st8], writes=[stg])
                S.op("pool", lambda e: e.tensor_tensor(out=stg[:, 0:6, :], in0=stg[:, 0:6, :], in1=gain8[:, 0:6, :], op=ALU.mult),
                     reads=[stg, gain8], writes=[stg])
                S.op("act", lambda e: e.copy(out=stg[:, 6:8, :], in_=pA[:, 384:512].rearrange("p (a d) -> p a d", d=64)),
                     reads=[pA], writes=[stg], conc=True)
                S.op("act", lambda e: e.copy(out=VA[:, t, :, 0:64], in_=pB[:, 0:128].rearrange("p (a d) -> p a d", d=64)),
                     reads=[pB], writes=[VA], conc=True)
                S.op("act", lambda e: e.activation(out=GA[:, t, :], in_=pB[:, 128:140], func=AF.Sigmoid),
                     reads=[pB], writes=[GA], conc=(t > 0))
                tsl = slice(t * 128, (t + 1) * 128)
                transpose_to(S, C, TV(ALLT[:, :, tsl], ALLT.res), lambda jj: (stg, stg[:, jj, :]), 6, width=64, pool=ps_tr)
                transpose_to(S, C, TV(RAW[:, :, tsl], RAW.res),
                             lambda jj: (stg, stg[:, 6:8, :].rearrange("p a d -> p (a d)")), 1, pool=ps_tr)

            nxt = proj_mm(0)
            for t in range(TPS):
                cur_ = nxt
                if t + 1 < TPS:
                    nxt = proj_mm(t + 1)
                proj_post(t, *cur_)
            hid = S.sb([64, 2, 127], F32, "hid")
            kcn2 = S.sb([128, 64], F32, "kcn2")
            junk = S.sb([128, 64], F32, "junk")
            stc = S.sb([128, 2], F32, "stc")
            for a in range(2):
                ph = C.psum.next()
                for l_ in range(32):
                    S.op("pe", lambda e, a=a, l_=l_, ph=ph, RAW=RAW: e.matmul(ph[0:64, 0:127], W1c[a * 64:(a + 1) * 64, l_, :],
                                                                            RAW[a * 64:(a + 1) * 64, 0, l_:l_ + 16 * 126 + 1:16],
                                                                            start=(l_ == 0), stop=(l_ == 31)), reads=[W1c, RAW], writes=[ph])
                S.op("act", lambda e, a=a, ph=ph, hid=hid: e.activation(out=hid[:, a, :], in_=ph[0:64, 0:127], func=AF.Silu, bias=CST[:, a:a + 1]),
                     reads=[ph, CST], writes=[hid], conc=(a > 0))
            pk = C.psum.next()
            for a in range(2):
                S.op("pe", lambda e, a=a, pk=pk, hid=hid: e.matmul(pk[0:127, a * 64:(a + 1) * 64], hid[:, a, :], W2c[:, a, :], start=True, stop=True),
                     reads=[hid, W2c], writes=[pk])
            S.op("pool", lambda e, stc=stc: e.memset(stc[:], 0.0), writes=[stc])
            S.op("act", lambda e, pk=pk, junk=junk, stc=stc: e.activation(out=junk[0:127, :], in_=pk[0:127, 0:64], func=AF.Square, accum_out=stc[0:127, 0:1]),
                 reads=[pk, stc], writes=[junk, stc])
            S.op("act", lambda e, stc=stc: e.activation(out=stc[0:127, 1:2], in_=stc[0:127, 0:1], func=AF.Sqrt, bias=EPS, scale=1.0 / 64),
                 reads=[stc], writes=[stc])
            S.op("dve", lambda e, stc=stc: e.reciprocal(out=stc[0:127, 1:2], in_=stc[0:127, 1:2]), reads=[stc], writes=[stc])
            S.op("dve", lambda e, pk=pk, stc=stc, kcn2=kcn2: e.scalar_tensor_tensor(out=kcn2[0:127, :], in0=pk[0:127, 0:64], scalar=stc[0:127, 1:2],
                                                                                in1=kg0B[0:127, :], op0=ALU.mult, op1=ALU.mult),
                 reads=[pk, stc, kg0B], writes=[kcn2])
            S.op("act", lambda e, pk=pk, VCA=VCA: e.copy(out=VCA[0:127, 0:64], in_=pk[0:127, 64:128]), reads=[pk], writes=[VCA], conc=True)
            ptk = C.psum.next()
            S.op("pe", lambda e, ptk=ptk, kcn2=kcn2: e.transpose(ptk[0:64, 0:127], kcn2[0:127, :], ident[0:127, 0:127]), reads=[kcn2, ident], writes=[ptk])
            S.op("act", lambda e, ptk=ptk, KCN=KCN: e.copy(out=KCN[0:64, :], in_=ptk[0:64, 0:127]), reads=[ptk], writes=[KCN], conc=True)
            S.release(mk_p)
            tmpp = Pool(S, 4, [128, 512], F32, "tmp")
            Ep = Pool(S, 4, [128, 512], F32, "E")
            cfp = Pool(S, 16, [128, 2], F32, "cf")
            evp = Pool(S, 16, [128, 97], F32, "ev")
            sc = S.sb([128, TPS, 32], F32, "sc")
            M8 = S.sb([128, TPS, 8], F32, "M8")
            NS = S.sb([32, T_SEQ], F32, "NS")

            pending = []

            def epilogue(po_, t, r, br, ncol, with_imp):
                cf = cfp.next()
                po = evp.next()
                S.op("act", lambda e: e.copy(out=po[:, 0:ncol], in_=po_[:, 0:ncol]), reads=[po_], writes=[po])

                def math():
                    S.op("dve", lambda e: e.tensor_scalar(out=cf[:, 0:1], in0=po[:, 64:65], scalar1=1e-30, scalar2=None, op0=ALU.max),
                         reads=[po], writes=[cf])
                    S.op("dve", lambda e: e.reciprocal(out=cf[:, 0:1], in_=cf[:, 0:1]), reads=[cf], writes=[cf])
                    if with_imp:
                        S.op("dve", lambda e: e.scalar_tensor_tensor(out=IMP[:, t, :], in0=po[:, 65:97], scalar=cf[:, 0:1], in1=IMP[:, t, :],
                                                                     op0=ALU.mult, op1=ALU.add), reads=[po, cf, IMP], writes=[IMP])
                    S.op("dve", lambda e: e.tensor_tensor(out=cf[:, 1:2], in0=cf[:, 0:1], in1=GA[:, t, r * 3 + br:r * 3 + br + 1], op=ALU.mult),
                         reads=[cf, GA], writes=[cf])
                    S.op("dve", lambda e: e.scalar_tensor_tensor(out=oacc[:, t, r, :], in0=po[:, 0:64], scalar=cf[:, 1:2], in1=oacc[:, t, r, :],
                                                                 op0=ALU.mult, op1=ALU.add), reads=[po, cf, oacc], writes=[oacc])
                pending.append(math)

            def flush_pending(n=None):
                k_ = len(pending) if n is None else min(n, len(pending))
                for _ in range(k_):
                    pending.pop(0)()

            pspool = Pool(S, 3, None, tiles=[C.psum.t[0], C.psum.t[1], C.psum.t[6]])
            po4 = C.psum.t[2:6]
            cbank = C.psum.t[7]

            def col_range(kind, Q, kt):
                if kind == "s":
                    return max(0, kt - 4 * Q) * 128, 512
                i = kt - (4 * Q - 4)
                if i <= 3:
                    return 0, (i + 1) * 128
                return (i - 4) * 128, 512

            started = {}

            def a_score(job, r, tabs):
                kind, Q, br, kidx, kts, kt = job
                qsl = slice(Q * 512, (Q + 1) * 512)
                ps = pspool.next()
                tmp = tmpp.next()
                E = Ep.next()
                if kind == "c":
                    SC = tabs["c"]
                    S.op("pe", lambda e: e.matmul(ps[0:127, :], KCN[:, :], ALLT[:, r, qsl], start=True, stop=True), reads=[KCN, ALLT], writes=[ps])
                    S.op("dve", lambda e: e.tensor_tensor(out=tmp[0:127, :], in0=ps[0:127, :], in1=SC[0:127, qsl], op=ALU.add),
                         reads=[ps, SC], writes=[tmp])
                    S.op("act", lambda e: e.activation(out=E[0:127, :], in_=tmp[0:127, :], func=AF.Exp), reads=[tmp], writes=[E])
                else:
                    tab = tabs[kind]
                    c0, c1 = col_range(kind, Q, kt)
                    ksl = slice(kt * 128, (kt + 1) * 128)
                    S.op("pe", lambda e: e.matmul(ps[:, c0:c1], ALLT[:, kidx, ksl], ALLT[:, r, Q * 512 + c0:Q * 512 + c1], start=True, stop=True),
                         reads=[ALLT], writes=[ps])
                    m0 = Q * 512 - kt * 128 + 512
                    S.op("dve", lambda e: e.tensor_tensor(out=tmp[:, c0:c1], in0=ps[:, c0:c1], in1=tab[:, m0 + c0:m0 + c1], op=ALU.add),
                         reads=[ps, tab], writes=[tmp])
                    S.op("act", lambda e: e.activation(out=E[:, c0:c1], in_=tmp[:, c0:c1], func=AF.Exp), reads=[tmp], writes=[E])
                return E

            def a_pv(job, E, r):
                kind, Q, br, kidx, kts, kt = job
                if kind == "c":
                    for qs in range(4):
                        S.op("pe", lambda e, qs=qs: e.matmul(cbank[:, qs * 100:qs * 100 + 97], E[0:127, qs * 128:(qs + 1) * 128], VCA[0:127, 0:97],
                                                         start=True, stop=True), reads=[E, VCA], writes=[cbank])
                    for qs in range(4):
                        epilogue(TV(cbank[:, qs * 100:qs * 100 + 97], cbank.res), Q * 4 + qs, r, 0, 97, True)
                    return
                c0, c1 = col_range(kind, Q, kt)
                if kt == kts[0]:
                    for qs in range(4):
                        started[qs] = False
                for qs in range(4):
                    if not (c0 <= qs * 128 < c1):
                        continue
                    first = not started[qs]
                    started[qs] = True
                    S.op("pe", lambda e, po=po4[qs], qs=qs, first=first, last=(kt == 4 * Q + qs): e.matmul(
                        po[:, 0:65], E[:, qs * 128:(qs + 1) * 128], VA[:, kt, br - 1, :], start=first, stop=last),
                        reads=[E, VA], writes=[po4[qs]])
                if kt == kts[-1]:
                    for qs in range(4):
                        epilogue(po4[qs], Q * 4 + qs, r, br, 65, False)

            def run_jobs(jobs, r, tabs, LA=2):
                Es = [a_score(jobs[i], r, tabs) for i in range(min(LA, len(jobs)))]
                for ji, job in enumerate(jobs):
                    if ji + LA < len(jobs):
                        Es.append(a_score(jobs[ji + LA], r, tabs))
                    flush_pending(1)
                    a_pv(job, Es.pop(0), r)
                flush_pending()

            mk_c = S.mark()
            SC = S.sb([128, 2048], F32, "SC")
            SW = S.sb([128, LW], F32, "SW")
            for r in range(4):
                h = 4 * g + r
                S.load(SC[:, :], dram_pitch(TC, h * (128 * (LC + 16) + LC) + 2017, LC, 128, 2048), reads=[TC], writes=[SC])
                S.load(SW[:], dram_pitch(TW, h * 128 * (LW + 1), LW, 128, LW), reads=[TW], writes=[SW])
                jobs = []
                for Q in range(4):
                    jobs.append(("c", Q, 0, None, None, None))
                    kts = list(range(max(0, 4 * Q - 4), 4 * Q + 4))
                    for kt in kts:
                        jobs.append(("w", Q, 2, 5, kts, kt))
                run_jobs(jobs, r, {"c": SC, "w": SW})
            S.release(mk_c)
            S.op("dve", lambda e: e.tensor_tensor(out=sc[:], in0=IMP[:], in1=cmnf[:], op=ALU.mult), reads=[IMP, cmnf], writes=[sc])
            S.op("dve", lambda e: e.tensor_tensor(out=sc[:], in0=sc[:], in1=sadd[:], op=ALU.add), reads=[sc, sadd], writes=[sc])
            for t in range(TPS):
                S.op("dve", lambda e, t=t: e.max(out=M8[:, t, :], in_=sc[:, t, :]), reads=[sc], writes=[M8], conc=(t > 0))
            S.op("dve", lambda e: e.tensor_tensor(out=sc[:], in0=sc[:], in1=M8[:, :, 7:8].broadcast_to([128, TPS, 32]), op=ALU.is_ge),
                 reads=[sc, M8], writes=[sc])
            S.op("dve", lambda e: e.tensor_scalar(out=sc[:], in0=sc[:], scalar1=-1.0, scalar2=-NEG, op0=ALU.add, op1=ALU.mult), reads=[sc], writes=[sc])
            for t4 in range(4):
                pb = C.psum.next()
                for q in range(4):
                    S.op("pe", lambda e, pb=pb, q=q, t4=t4: e.transpose(pb[0:32, q * 128:(q + 1) * 128], sc[:, t4 * 4 + q, :], ident[:]),
                         reads=[sc, ident], writes=[pb])
                S.op("act", lambda e, pb=pb, t4=t4: e.copy(out=NS[:, t4 * 512:(t4 + 1) * 512], in_=pb[0:32, :]), reads=[pb], writes=[NS], conc=(t4 > 0))
            for r in range(4):
                S.load(ALLT[64:96, r, :], NS[:], reads=[NS], writes=[ALLT], conc=(r > 0))
            SSs = [S.sb([128, LS], F32, f"SS{i_}") for i_ in range(2)]

            def load_ss(r):
                h = 4 * g + r
                S.load(SSs[r % 2][:], dram_pitch(TS, h * 128 * (LS + 1), LS, 128, LS), reads=[TS], writes=[SSs[r % 2]])

            load_ss(0)
            for r in range(4):
                if r + 1 < 4:
                    load_ss(r + 1)
                jobs = []
                for Q in range(4):
                    kts = list(range(0, 4 * Q + 4))
                    for kt in kts:
                        jobs.append(("s", Q, 1, 4, kts, kt))
                run_jobs(jobs, r, {"s": SSs[r % 2]})
            r0 = b * T_SEQ
            S.load(obuf.h[r0:r0 + T_SEQ, g * 256:(g + 1) * 256].rearrange("(t p) c -> p t c", p=128), oacc[:].rearrange("p t r d -> p t (r d)"),
                   reads=[oacc], writes=[obuf], conc=not (b == 0 and g == 0))
            S.release(mk_g)
    S.release(mk_phase)
    outproj_phase(S, C, ins, ins["nsa_w_out"][j], obuf, xin, xout)


class XBuf:
    def __init__(self, t):
        self.h = t.h
        self.t = t
        self.tiles = [Res(f"xt{i}") for i in range(NT)]


W_SHAPES = {
    "ada_w": [2, 2, 1024, 3072], "ada_b": [2, 2, 3072], "norm_g": [2, 2, 1024], "rel_bias": [32, 16],
    "nsa_w_in": [1, 1024, 2608], "nsa_q_gain": [1, 64], "nsa_k_gain": [1, 3, 64], "nsa_cmp_pe": [1, 2, 32, 64],
    "nsa_cmp_w1": [1, 2, 2048, 64], "nsa_cmp_w2": [1, 2, 64, 64], "nsa_w_out": [1, 1024, 1024],
    "hgrn_w_in": [1, 1024, 4096], "hgrn_lower_bounds": [2, 1024], "hgrn_out_gain": [1, 128],
    "hgrn_w_out": [1, 1024, 1024], "moe_router_group": [2, 1024, 4], "moe_router_expert": [2, 1024, 16],
    "moe_w1": [2, 16, 1024, 512], "moe_w3": [2, 16, 1024, 512], "moe_w2": [2, 16, 512, 1024],
}

ALL_SUBLAYERS = [(0, 0), (0, 1), (1, 0), (1, 1)]


def build(sublayers=ALL_SUBLAYERS):
    nc = bass.Bass("TRN2", target_bir_lowering=False)
    S = Sch(nc)
    C = Ctx()
    ins = {}
    ins["x"] = S.dram("x", [NTOK, D], kind="ExternalInput")
    ins["c"] = S.dram("c", [NB, D], kind="ExternalInput")
    for name, shp in W_SHAPES.items():
        ins[name] = S.dram(name, shp, kind="ExternalInput")
    for name, arr in host_consts().items():
        ins[name] = S.dram(name, list(arr.shape), I32 if arr.dtype == np.int32 else F32, kind="ExternalInput")
    out = XBuf(S.dram("out", [NTOK, D], kind="ExternalOutput"))
    scr = [XBuf(S.dram(f"xs{i}", [NTOK, D])) for i in range(2)]
    setup_common(S, C, ins)
    cur = XBuf(ins["x"])
    n = len(sublayers)
    for i, (l, s) in enumerate(sublayers):
        dst = out if i == n - 1 else scr[i % 2]
        if s == 1:
            moe_sublayer(S, C, ins, l, cur, dst)
        elif l % 2 == 0:
            nsa_sublayer(S, C, ins, l, cur, dst)
        else:
            hgrn_sublayer(S, C, ins, l, cur, dst)
        cur = dst
    S.finish()
    S.emit()
    return nc


def kernel(**inputs):
    inputs = {k: np.asarray(v) for k, v in inputs.items()}
    nc = build()
    hc = host_consts()
    x = np.ascontiguousarray(inputs["x"], dtype=np.float32)
    c = np.ascontiguousarray(inputs["c"], dtype=np.float32)
    in_maps = []
    for core in range(N_CORES):
        m = {"x": x[core * NB:(core + 1) * NB].reshape(NTOK, D), "c": c[core * NB:(core + 1) * NB]}
        for name in W_SHAPES:
            m[name] = np.ascontiguousarray(inputs[name], dtype=np.float32)
        m.update(hc)
        in_maps.append(m)
    res = run_bass_kernel_spmd(nc, in_maps, core_ids=list(range(N_CORES)))
    outs = [r["out"].reshape(NB, T_SEQ, D) for r in res.results]
    return np.concatenate(outs, axis=0).astype(np.float32)
```

```python
from contextlib import ExitStack
import numpy as np
import concourse.bass as bass
import concourse.mybir as mybir
from concourse.ap import AP
from concourse.bass_utils import run_bass_kernel_spmd

F32 = mybir.dt.float32
BF16 = mybir.dt.bfloat16
I32 = mybir.dt.int32
ALU = mybir.AluOpType
AF = mybir.ActivationFunctionType
AX = mybir.AxisListType

ENGS = ["pe", "act", "dve", "pool", "sp"]


class Res:
    __slots__ = ("name", "w", "r", "xw", "excl")

    def __init__(self, name=""):
        self.name = name
        self.excl = False
        self.xw = {}
        self.w = {}
        self.r = {}


class T:
    def __init__(self, h, res=None, name=""):
        self.h = h
        self.res = res if res is not None else Res(name)

    def __getitem__(self, k):
        return self.h[k]

    def ap(self):
        return self.h.ap() if hasattr(self.h, "ap") and callable(self.h.ap) else self.h[:]


def _res(x):
    if isinstance(x, T):
        return x.res
    return x


class Sch:
    def __init__(self, nc, n_dma_sems=28, same_engine_sync=True):
        self.nc = nc
        self.stack = ExitStack()
        self.items = {e: [] for e in ENGS}
        self.count = {e: 0 for e in ENGS}
        self.esem = {e: self.stack.enter_context(nc.semaphore(f"s_{e}")) for e in ENGS}
        self.dsem = [self.stack.enter_context(nc.semaphore(f"sd{i}")) for i in range(n_dma_sems)]
        self.dcount = [0] * n_dma_sems
        self.n_hw = 16
        self.dnext = {"hw": 0, "sw": 0}
        self.seen = {e: {} for e in ENGS}
        self.same = same_engine_sync
        self.nid = 0
        self.SB_WORDS = 53000
        self.big = self.stack.enter_context(nc.sbuf_tensor("bigsb", [128, self.SB_WORDS], F32))
        self.sb_off = 0
        self.sb_peak = 0

    def sb(self, shape, dtype=F32, name=None):
        shape = list(shape)
        n = int(np.prod(shape[1:]))
        n_al = (n + 7) // 8 * 8
        assert self.sb_off + n_al <= self.SB_WORDS, f"SBUF carve overflow at {name}: {self.sb_off}+{n_al}"
        v = self.big[0:shape[0], self.sb_off:self.sb_off + n]
        self.sb_off += n_al
        self.sb_peak = max(self.sb_peak, self.sb_off)
        if dtype != F32:
            v = v.bitcast(dtype)
        if len(shape) == 3:
            v = v.rearrange("p (a b) -> p a b", b=shape[2])
        elif len(shape) == 4:
            v = v.rearrange("p (a b c) -> p a b c", b=shape[2], c=shape[3])
        return T(v, name=name or "t")

    def mark(self):
        return self.sb_off

    def release(self, mark):
        self.barrier()
        self.sb_off = mark

    def barrier(self):
        for eng in ENGS:
            waits = []
            seen = self.seen[eng]
            for i, c in enumerate(self.dcount):
                if c and seen.get(("d", i), 0) < 16 * c:
                    seen[("d", i)] = 16 * c
                    waits.append((self.dsem[i], 16 * c))
            for e in ENGS:
                if e != eng and self.count[e] and seen.get(("e", e), 0) < self.count[e]:
                    seen[("e", e)] = self.count[e]
                    waits.append((self.esem[e], self.count[e]))
            if waits:
                self.items[eng].append((waits, None, None, 0))

    def ps(self, shape, dtype=F32, name=None):
        self.nid += 1
        name = name or f"p{self.nid}"
        h = self.stack.enter_context(self.nc.psum_tensor(f"{name}_{self.nid}", list(shape), dtype))
        t = T(h, name=name)
        t.res.excl = True
        return t

    def dram(self, name, shape, dtype=F32, kind="Internal"):
        h = self.nc.dram_tensor(name, list(shape), dtype, kind=kind)
        return T(h, name=name)

    def _sem(self, key):
        return self.esem[key[1]] if key[0] == "e" else self.dsem[key[1]]

    def _deps(self, eng, reads, writes, conc=False):
        deps = {}

        def add(tok):
            if tok is None:
                return
            key, val = tok
            if key[0] == "e" and key[1] == eng:
                if eng == "pe" or not self.same:
                    return
            if deps.get(key, 0) < val:
                deps[key] = val

        for r in reads:
            for key, val in _res(r).w.items():
                add((key, val))
        for w in writes:
            rr = _res(w)
            for key, val in (rr.xw if conc else rr.w).items():
                add((key, val))
            for key, val in rr.r.items():
                add((key, val))
        waits = []
        seen = self.seen[eng]
        for key, val in deps.items():
            if seen.get(key, 0) >= val:
                continue
            seen[key] = val
            waits.append((self._sem(key), val))
        return waits

    def _commit(self, tok, reads, writes, conc=False):
        key, val = tok
        for r in reads:
            rr = _res(r)
            if rr.r.get(key, 0) < val:
                rr.r[key] = val
        for w in writes:
            rr = _res(w)
            if conc:
                if rr.w.get(key, 0) < val:
                    rr.w[key] = val
            else:
                rr.w = {key: val}
                rr.xw = {key: val}
                rr.r = {}

    def op(self, eng, fn, reads=(), writes=(), conc=False):
        ex = [r for r in reads if _res(r).excl]
        if ex:
            assert not conc or all(not _res(w).excl for w in writes)
            if conc:
                w0 = self._deps(eng, [], ex, False)
                self.items[eng].append((w0, None, None, 0)) if w0 else None
                reads = [r for r in reads if not _res(r).excl]
                exq = ex
            else:
                writes = list(writes) + ex
                reads = [r for r in reads if not _res(r).excl]
                exq = []
        else:
            exq = []
        waits = self._deps(eng, reads, writes, conc)
        self.count[eng] += 1
        tok = (("e", eng), self.count[eng])
        self.items[eng].append((waits, fn, self.esem[eng], 1))
        self._commit(tok, reads, writes, conc)
        if exq:
            self._commit(tok, [], exq, False)
        return tok

    def dma(self, eng, fn, reads=(), writes=(), conc=False):
        waits = self._deps(eng, reads, writes, conc)
        if eng == "pool":
            i = self.n_hw + self.dnext["sw"]
            self.dnext["sw"] = (self.dnext["sw"] + 1) % (len(self.dsem) - self.n_hw)
        else:
            i = self.dnext["hw"]
            self.dnext["hw"] = (self.dnext["hw"] + 1) % self.n_hw
        if self.dcount[i] > 0:
            key = ("d", i)
            val = 16 * self.dcount[i]
            if self.seen[eng].get(key, 0) < val:
                self.seen[eng][key] = val
                waits.append((self.dsem[i], val))
        self.dcount[i] += 1
        tok = (("d", i), 16 * self.dcount[i])
        self.items[eng].append((waits, fn, self.dsem[i], 16))
        self._commit(tok, reads, writes, conc)
        return tok

    def load(self, dst_ap, src_ap, reads=(), writes=(), eng="sp", conc=False, **kw):
        return self.dma(eng, lambda e: e.dma_start(out=dst_ap, in_=src_ap, **kw), reads, writes, conc)

    def finish(self, eng="sp"):
        waits = []
        for i, c in enumerate(self.dcount):
            if c:
                waits.append((self.dsem[i], 16 * c))
        for e in ENGS:
            if e != eng and self.count[e]:
                waits.append((self.esem[e], self.count[e]))
        self.items[eng].append((waits, None, None, 0))

    def emit(self):
        nc = self.nc
        items = self.items

        def replay(name, e):
            for waits, fn, sem, inc in items[name]:
                for s, v in waits:
                    e.wait_ge(s, v)
                if fn is not None:
                    ins = fn(e)
                    ins.then_inc(sem, inc)

        with nc.Block() as block:
            @block.sync
            def _(e):
                replay("sp", e)

            @block.tensor
            def _(e):
                replay("pe", e)

            @block.scalar
            def _(e):
                replay("act", e)

            @block.vector
            def _(e):
                replay("dve", e)

            @block.gpsimd
            def _(e):
                replay("pool", e)
        self.stack.close()


D = 1024
T_SEQ = 2048
NB = 2
NTOK = NB * T_SEQ
NT = NTOK // 128
TPS = T_SEQ // 128
EPS = 1e-6
NBLK = 80
N_CORES = 8


_BKT = {}


def t5_bucket_table(n):
    if n in _BKT:
        return _BKT[n]
    import math
    tab = None
    try:
        import jax
        import jax.numpy as jnp
        with jax.default_device(jax.devices("cpu")[0]):
            d = jnp.arange(n)
            nf = jnp.maximum(d, 1).astype(jnp.float32)
            large = 16 + (jnp.log(nf / 16) / math.log(1024 / 16) * 16).astype(jnp.int32)
            large = jnp.minimum(large, 31)
            tab = np.asarray(jnp.where(d < 16, d, large)).astype(np.int64)
    except Exception:
        tab = None
    if tab is None:
        d = np.arange(n)
        nf = np.maximum(d, 1).astype(np.float32)
        large = 16 + (np.log(nf / np.float32(16)) / np.float32(math.log(1024 / 16)) * np.float32(16)).astype(np.int32)
        large = np.minimum(large, 31)
        tab = np.where(d < 16, d, large).astype(np.int64)
    _BKT[n] = tab
    return tab


def host_consts():
    c = {}
    i = np.arange(128)
    c["ident"] = np.eye(128, dtype=np.float32)
    c["ones"] = np.ones((128, 128), np.float32)
    c["ustrict"] = (i[:, None] < i[None, :]).astype(np.float32)
    c["thr"] = np.tile((128.0 * np.arange(NBLK, dtype=np.float32))[None, :], (128, 1))
    c["piota"] = i.astype(np.float32).reshape(128, 1)
    seg = np.arange(2 * NT)
    tok = (seg[None, :] // 2) * 128 + i[:, None]
    c["tokid"] = tok.astype(np.int32)
    c["aid"] = (2 * tok + (seg[None, :] % 2)).astype(np.int32)
    init3 = np.zeros((128, NBLK, 3), np.int32)
    init3[:, :, 2] = 2 * NTOK + i[:, None] * NBLK + np.arange(NBLK)[None, :]
    c["init3"] = init3
    sel = np.zeros((2, 2, 128), np.float32)
    sel[0, 0, :] = 1.0
    sel[1, 1, :] = 1.0
    c["sel2"] = sel.reshape(2, 256)
    same = (i[:, None] // 32) == (i[None, :] // 32)
    c["ltri"] = (same & (i[:, None] <= i[None, :])).astype(np.float32)
    c["urev"] = (same & (i[:, None] > i[None, :])).astype(np.float32)
    cm = ((i[:, None] // 32) == np.arange(4)[None, :]).astype(np.float32)
    c["cm"] = cm
    c["cmrow"] = np.tile(cm.T.reshape(1, 512), (128, 1)).astype(np.float32)
    t = np.arange(TPS)[None, :, None] * 128 + i[:, None, None]
    cur = t // 64
    blk = np.arange(32)[None, None, :]
    forced = (blk == 0) | (blk == cur) | (blk == cur - 1)
    causal = blk <= cur
    c["cmnf"] = (causal & ~forced).astype(np.float32)
    c["sadd"] = (1.0e4 * forced - 1.0 * (~causal)).astype(np.float32)
    kt = np.arange(TPS)[None, :, None]
    c["ex"] = (((kt * 128 + np.arange(128)[None, None, :]) // 64) == np.arange(32)[:, None, None]).astype(np.float32)
    c["bkoh"] = (t5_bucket_table(T_SEQ)[None, :] == np.arange(32)[:, None]).astype(np.float32)
    cs = np.arange(127) * 16
    ss_ = np.arange(32) * 64
    shared = np.minimum(cs[:, None] + 32, ss_[None, :] + 64) - np.maximum(cs[:, None], ss_[None, :])
    c["c2s"] = (np.clip(shared, 0, None) / 32).astype(np.float32)
    return c


class Pool:
    def __init__(self, S, n, shape, dtype=F32, name="pool", psum=False, tiles=None):
        self.t = tiles if tiles is not None else [(S.ps if psum else S.sb)(shape, dtype, f"{name}{i}") for i in range(n)]
        self.i = 0

    def next(self):
        t = self.t[self.i]
        self.i = (self.i + 1) % len(self.t)
        return t


class Ctx:
    pass


LATE_CONSTS = ("cmnf", "sadd", "ex", "bkoh", "c2s", "sel2")
NEG = -30000.0
OOB_IDX = 65536.0


DEBUG = False


def dump(S, name, t, shape, dtype=F32):
    if not DEBUG:
        return
    d = S.dram("dbg_" + name, list(shape), dtype, kind="ExternalOutput")
    S.load(d.ap(), t[:], reads=[t], writes=[d])


def setup_common(S, C, ins):
    hc = host_consts()
    C.k = {}
    for name, arr in hc.items():
        if name in LATE_CONSTS:
            continue
        dt = I32 if arr.dtype == np.int32 else F32
        t = S.sb(list(arr.shape), dt, name)
        S.load(t[:], ins[name].ap(), reads=[ins[name]], writes=[t])
        C.k[name] = t
    C.psum = Pool(S, 8, [128, 512], F32, "bank", psum=True)
    cT = S.sb([128, 8, 2], F32, "cT")
    for b in range(2):
        S.load(cT[:, :, b], ins["c"][b, :].rearrange("(k p) -> p k", p=128), reads=[ins["c"]], writes=[cT],
               conc=(b > 0), allow_slow_non_contiguous=True)
    C.condT = S.sb([128, 8, 2], F32, "condT")
    S.op("act", lambda e: e.activation(out=C.condT[:], in_=cT[:], func=AF.Silu), reads=[cT], writes=[C.condT])
    C.G = [S.sb([128, 1024], F32, f"G{b}") for b in range(2)]
    C.Sh = [S.sb([128, 1024], F32, f"Sh{b}") for b in range(2)]
    C.Gt = [S.sb([128, 1024], F32, f"Gt{b}") for b in range(2)]
    C.xpool = Pool(S, 2, [128, 1024], F32, "xt")
    C.hpool = Pool(S, 2, [128, 1024], F32, "ht")
    C.sqpool = Pool(S, 1, [128, 1024], F32, "sq")
    C.hTpool = Pool(S, 2, [128, 8, 128], F32, "hT")
    C.stat = Pool(S, 4, [128, 2], F32, "stat")


def mod_tiles(S, C, ins, l, s):
    ada_w = ins["ada_w"]
    mk = S.mark()
    wpool = Pool(S, 2, [128, 8, 384], F32, "wmod")
    modT = S.sb([128, 24, 2], F32, "modT")
    bT = S.sb([128, 24], F32, "bT")
    gT = S.sb([128, 8], F32, "gT")
    dg = Pool(S, 2, [128, 128], F32, "diag")
    S.load(bT[:], ins["ada_b"][l, s, :].rearrange("(e p) -> p e", p=128), reads=[ins["ada_b"]], writes=[bT],
           allow_slow_non_contiguous=True)
    S.load(gT[:], ins["norm_g"][l, s, :].rearrange("(e p) -> p e", p=128), reads=[ins["norm_g"]], writes=[gT],
           allow_slow_non_contiguous=True)
    acc = C.psum.next()
    for cb in range(8):
        w = wpool.next()
        S.load(w[:], ada_w[l, s, :, cb * 384:(cb + 1) * 384].rearrange("(k p) n -> p k n", p=128), reads=[ada_w], writes=[w])
        for e in range(3):
            ee = cb * 3 + e
            for k in range(8):
                S.op("pe", lambda e_, e=e, ee=ee, k=k, w=w: e_.matmul(acc[:, ee * 2:ee * 2 + 2], w[:, k, e * 128:(e + 1) * 128], C.condT[:, k, :],
                                                                  start=(k == 0), stop=(k == 7)),
                     reads=[C.condT, w], writes=[acc])
    S.op("dve", lambda e: e.tensor_tensor(out=modT[:], in0=acc[:, 0:48].rearrange("p (e b) -> p e b", b=2),
                                          in1=bT[:].unsqueeze(2).broadcast_to([128, 24, 2]), op=ALU.add), reads=[acc, bT], writes=[modT])
    S.op("dve", lambda e: e.scalar_tensor_tensor(out=modT[:, 8:16, :], in0=modT[:, 8:16, :], scalar=1.0,
                                                 in1=gT[:].unsqueeze(2).broadcast_to([128, 8, 2]), op0=ALU.add, op1=ALU.mult),
         reads=[modT, gT], writes=[modT])
    dst = [C.Sh, C.G, C.Gt]
    ident = C.k["ident"]
    ones = C.k["ones"]
    for b in range(2):
        for which in range(3):
            for half in range(2):
                pb = C.psum.next()
                for q in range(4):
                    ee = which * 8 + half * 4 + q
                    d = dg.next()
                    S.op("dve", lambda e, d=d, ee=ee, b=b: e.tensor_scalar(out=d[:], in0=ident[:], scalar1=modT[:, ee, b:b + 1], scalar2=None,
                                                                         op0=ALU.mult), reads=[ident, modT], writes=[d])
                    S.op("pe", lambda e, d=d, q=q, pb=pb: e.matmul(pb[:, q * 128:(q + 1) * 128], ones[:], d[:], start=True, stop=True),
                         reads=[ones, d], writes=[pb])
                tgt = dst[which][b]
                S.op("act", lambda e, tgt=tgt, half=half, pb=pb: e.copy(out=tgt[:, half * 512:(half + 1) * 512], in_=pb[:, :]),
                     reads=[pb], writes=[tgt], conc=(half > 0))
    S.release(mk)


def norm_tile(S, C, xt, b):
    sq = C.sqpool.next()
    st = C.stat.next()
    S.op("pool", lambda e: e.memset(st[:], 0.0), writes=[st])
    S.op("act", lambda e: e.activation(out=sq[:], in_=xt[:], func=AF.Square, accum_out=st[:, 0:1]),
         reads=[xt, st], writes=[sq, st])
    S.op("act", lambda e: e.activation(out=st[:, 1:2], in_=st[:, 0:1], func=AF.Sqrt, bias=EPS, scale=1.0 / D),
         reads=[st], writes=[st])
    S.op("dve", lambda e: e.reciprocal(out=st[:, 1:2], in_=st[:, 1:2]), reads=[st], writes=[st])
    h = C.hpool.next()
    S.op("dve", lambda e: e.scalar_tensor_tensor(out=h[:], in0=xt[:], scalar=st[:, 1:2], in1=C.G[b][:],
                                                 op0=ALU.mult, op1=ALU.mult), reads=[xt, st, C.G[b]], writes=[h])
    S.op("dve", lambda e: e.tensor_tensor(out=h[:], in0=h[:], in1=C.Sh[b][:], op=ALU.add), reads=[h, C.Sh[b]], writes=[h])
    return h


def transpose_to(S, C, dst, src_fn, n, width=128, rows=128, evac="act", pool=None):
    ident = C.k["ident"]
    j = 0
    first = True
    while j < n:
        m = min(4, n - j)
        pb = (pool or C.psum).next()
        for q in range(m):
            S.op("pe", lambda e, q=q, jj=j + q, pb=pb: e.transpose(pb[0:width, q * rows:(q + 1) * rows], src_fn(jj)[1],
                                                                 ident[0:rows, 0:rows]),
                 reads=[src_fn(j + q)[0], ident], writes=[pb])
        dv = dst[0:width, j:j + m, :]
        src = pb[0:width, 0:m * rows].rearrange("p (a b) -> p a b", b=rows)
        if evac == "act":
            S.op("act", lambda e, dv=dv, src=src: e.copy(out=dv, in_=src), reads=[pb], writes=[dst], conc=not first)
        else:
            S.op("dve", lambda e, dv=dv, src=src: e.tensor_copy(out=dv, in_=src), reads=[pb], writes=[dst], conc=not first)
        first = False
        j += m


def bc_reg(S, e):
    if getattr(S, "_bc_reg", None) is None:
        r = e.alloc_register("bc_rows")
        e.reg_mov(r, 4095)
        S._bc_reg = r
    return S._bc_reg


def moe_sublayer(S, C, ins, l, xin, xout, dbg=None):
    mod_tiles(S, C, ins, l, 1)
    k = C.k
    mk_moe = S.mark()
    if True:
        M = Ctx()
        if not hasattr(C, "moe_dram"):
            kd = "ExternalOutput" if DEBUG else "Internal"
            C.moe_dram = (S.dram("hbuf", [NTOK, D], kind=kd), S.dram("asg", [NBLK * 128, 3], I32, kind=kd),
                          S.dram("ybuf", [2 * NTOK + NBLK * 128, D], kind=kd))
        M.Wr = S.sb([128, 8, 20], F32, "Wr")
        M.LG = S.sb([128, NT, 20], F32, "LG")
        M.OH = S.sb([128, 2 * NT, 16], F32, "OH")
        M.Wt = S.sb([128, 2 * NT], F32, "Wt")
        M.t4 = [S.sb([128, NT, 4], F32, f"t4_{i}") for i in range(6)]
        M.t1 = [S.sb([128, NT], F32, f"t1_{i}") for i in range(8)]
        M.t16 = S.sb([128, NT, 16], F32, "t16")
        M.cnt = S.sb([128, 2 * NT, 16], F32, "cnt")
        M.pp = [S.sb([128, 2 * NT, 16], F32, f"pp{i}") for i in range(2)]
        M.e16 = [S.sb([128, 16], F32, f"e16_{i}") for i in range(6)]
        M.dest = S.sb([128, 2 * NT], F32, "dest")
        M.desti = S.sb([128, 2 * NT], I32, "desti")
        M.cmp = S.sb([128, NBLK, 16], F32, "cmp")
        M.blke = S.sb([128, NBLK], F32, "blke")
        M.idxw = S.sb([128, NBLK], I32, "idxw")
        M.info = S.sb([128, 2 * NT, 3], I32, "info")
        M.hbuf, M.asg, M.ybuf = C.moe_dram
        M.ab = Pool(S, 3, [128, 3], I32, "ab")
        M.xg = C.xpool
        M.xgT = C.hTpool
    S.load(M.Wr[:, :, 0:4], ins["moe_router_group"][l].rearrange("(k p) g -> p k g", p=128),
           reads=[ins["moe_router_group"]], writes=[M.Wr], allow_slow_non_contiguous=True)
    S.load(M.Wr[:, :, 4:20], ins["moe_router_expert"][l].rearrange("(k p) g -> p k g", p=128),
           reads=[ins["moe_router_expert"]], writes=[M.Wr], conc=True, allow_slow_non_contiguous=True)

    def p1_a(i):
        b = i // TPS
        xt = C.xpool.next()
        S.load(xt[:], xin.h[i * 128:(i + 1) * 128, :], reads=[xin.tiles[i]], writes=[xt])
        h = norm_tile(S, C, xt, b)
        S.load(M.hbuf.h[i * 128:(i + 1) * 128, :], h[:], reads=[h], writes=[M.hbuf], conc=(i > 0))
        return h

    def p1_b(i, h):
        hT = C.hTpool.next()
        transpose_to(S, C, hT, lambda j: (h, h[:, j * 128:(j + 1) * 128]), 8)
        pb = C.psum.next()
        for kk in range(8):
            S.op("pe", lambda e, kk=kk: e.matmul(pb[:, 0:20], hT[:, kk, :], M.Wr[:, kk, :], start=(kk == 0), stop=(kk == 7)),
                 reads=[hT, M.Wr], writes=[pb])
        S.op("act", lambda e: e.copy(out=M.LG[:, i, :], in_=pb[:, 0:20]), reads=[pb], writes=[M.LG], conc=(i > 0))

    hn = p1_a(0)
    for i in range(NT):
        hc_ = hn
        if i + 1 < NT:
            hn = p1_a(i + 1)
        p1_b(i, hc_)

    LG = M.LG
    lgg = LG[:, :, 0:4]
    mg, sg, m1, m2, dd, r1, w1, w2 = M.t1
    ohg, eg, el, oh1, el2, oh2 = M.t4
    V = lambda t: t[:]

    def bc4(t):
        return t[:].unsqueeze(2).broadcast_to([128, NT, 4])

    def dve(fn, reads, writes):
        S.op("dve", fn, reads=reads, writes=writes)

    dve(lambda e: e.tensor_reduce(out=mg[:], in_=lgg, axis=AX.X, op=ALU.max), [LG], [mg])
    dve(lambda e: e.tensor_tensor(out=ohg[:], in0=lgg, in1=bc4(mg), op=ALU.is_ge), [LG, mg], [ohg])
    dve(lambda e: e.tensor_tensor(out=eg[:], in0=lgg, in1=bc4(mg), op=ALU.subtract), [LG, mg], [eg])
    S.op("act", lambda e: e.activation(out=eg[:], in_=eg[:], func=AF.Exp), reads=[eg], writes=[eg])
    dve(lambda e: e.tensor_reduce(out=sg[:], in_=eg[:], axis=AX.X, op=ALU.add), [eg], [sg])
    dve(lambda e: e.reciprocal(out=sg[:], in_=sg[:]), [sg], [sg])
    dve(lambda e: e.tensor_tensor(out=M.t16[:].rearrange("p t (g j) -> p t g j", j=4),
                                  in0=LG[:, :, 4:20].rearrange("p t (g j) -> p t g j", j=4),
                                  in1=ohg[:].unsqueeze(3).broadcast_to([128, NT, 4, 4]), op=ALU.mult), [LG, ohg], [M.t16])
    dve(lambda e: e.tensor_reduce(out=el[:], in_=M.t16[:].rearrange("p t (g j) -> p t j g", j=4), axis=AX.X, op=ALU.add),
        [M.t16], [el])
    dve(lambda e: e.tensor_reduce(out=m1[:], in_=el[:], axis=AX.X, op=ALU.max), [el], [m1])
    dve(lambda e: e.tensor_tensor(out=oh1[:], in0=el[:], in1=bc4(m1), op=ALU.is_ge), [el, m1], [oh1])
    dve(lambda e: e.scalar_tensor_tensor(out=el2[:], in0=oh1[:], scalar=-1e30, in1=el[:], op0=ALU.mult, op1=ALU.add),
        [oh1, el], [el2])
    dve(lambda e: e.tensor_reduce(out=m2[:], in_=el2[:], axis=AX.X, op=ALU.max), [el2], [m2])
    dve(lambda e: e.tensor_tensor(out=oh2[:], in0=el2[:], in1=bc4(m2), op=ALU.is_ge), [el2, m2], [oh2])
    dve(lambda e: e.tensor_tensor(out=dd[:], in0=m2[:], in1=m1[:], op=ALU.subtract), [m1, m2], [dd])
    S.op("act", lambda e: e.activation(out=dd[:], in_=dd[:], func=AF.Exp), reads=[dd], writes=[dd])
    dve(lambda e: e.tensor_scalar(out=r1[:], in0=dd[:], scalar1=1.0, scalar2=None, op0=ALU.add), [dd], [r1])
    dve(lambda e: e.reciprocal(out=r1[:], in_=r1[:]), [r1], [r1])
    dve(lambda e: e.tensor_tensor(out=w1[:], in0=sg[:], in1=r1[:], op=ALU.mult), [sg, r1], [w1])
    dve(lambda e: e.tensor_tensor(out=w2[:], in0=sg[:], in1=w1[:], op=ALU.subtract), [sg, w1], [w2])
    OHv = M.OH[:].rearrange("p (t s) e -> p t s e", s=2)
    Wtv = M.Wt[:].rearrange("p (t s) -> p t s", s=2)
    for s_, oh, w in ((0, oh1, w1), (1, oh2, w2)):
        dve(lambda e, s_=s_, oh=oh: e.tensor_tensor(
            out=OHv[:, :, s_, :].rearrange("p t (g j) -> p t g j", j=4),
            in0=ohg[:].unsqueeze(3).broadcast_to([128, NT, 4, 4]),
            in1=oh[:].unsqueeze(2).broadcast_to([128, NT, 4, 4]), op=ALU.mult), [ohg, oh], [M.OH])
        dve(lambda e, s_=s_, w=w: e.tensor_copy(out=Wtv[:, :, s_], in_=w[:]), [w], [M.Wt])

    OHf = M.OH[:].rearrange("p s e -> p (s e)")
    rank = [C.psum.next() for _ in range(2)]
    cntp = [C.psum.next() for _ in range(2)]
    for hh in range(2):
        S.op("pe", lambda e, hh=hh: e.matmul(rank[hh][:, :], k["ustrict"][:], OHf[:, hh * 512:(hh + 1) * 512], start=True, stop=True),
             reads=[k["ustrict"], M.OH], writes=[rank[hh]])
        S.op("pe", lambda e, hh=hh: e.matmul(cntp[hh][:, :], k["ones"][:], OHf[:, hh * 512:(hh + 1) * 512], start=True, stop=True),
             reads=[k["ones"], M.OH], writes=[cntp[hh]])
    cntf = M.cnt[:].rearrange("p s e -> p (s e)")
    for hh in range(2):
        S.op("act", lambda e, hh=hh: e.copy(out=cntf[:, hh * 512:(hh + 1) * 512], in_=cntp[hh][:, :]),
             reads=[cntp[hh]], writes=[M.cnt], conc=(hh > 0))
    cur = M.cnt
    NS = 2 * NT
    sh = 1
    pi = 0
    while sh < NS:
        nxt = M.pp[pi]
        pi ^= 1
        S.op("pool", lambda e, cur=cur, nxt=nxt, sh=sh: e.tensor_copy(out=nxt[:, 0:sh, :], in_=cur[:, 0:sh, :]), reads=[cur], writes=[nxt])
        dve(lambda e, cur=cur, nxt=nxt, sh=sh: e.tensor_tensor(out=nxt[:, sh:NS, :], in0=cur[:, sh:NS, :], in1=cur[:, 0:NS - sh, :], op=ALU.add),
            [cur], [nxt])
        cur = nxt
        sh *= 2
    incl = cur
    tot, t127, md, padded, pe_a, pe_b = M.e16
    cmpv = M.cmp[:].rearrange("p j e -> p (j e)").rearrange("p (e j) -> p e j", j=NBLK)
    dve(lambda e: e.tensor_tensor(out=cmpv, in0=incl[:, NS - 1, :].unsqueeze(2).broadcast_to([128, 16, NBLK]),
                                  in1=k["thr"][:].unsqueeze(1).broadcast_to([128, 16, NBLK]), op=ALU.is_gt), [incl, k["thr"]], [M.cmp])
    dve(lambda e: e.tensor_reduce(out=md[:], in_=cmpv, axis=AX.X, op=ALU.add), [M.cmp], [md])
    dve(lambda e: e.tensor_scalar(out=padded[:], in0=md[:], scalar1=128.0, scalar2=None, op0=ALU.mult), [md], [padded])
    a, bb = padded, pe_a
    sh = 1
    while sh < 16:
        dst = pe_a if a is not pe_a else pe_b
        dve(lambda e, a=a, dst=dst, sh=sh: e.tensor_copy(out=dst[:, 0:sh], in_=a[:, 0:sh]), [a], [dst])
        dve(lambda e, a=a, dst=dst, sh=sh: e.tensor_tensor(out=dst[:, sh:16], in0=a[:, sh:16], in1=a[:, 0:16 - sh], op=ALU.add), [a], [dst])
        a = dst
        sh *= 2
    pad_end = a
    pad_start = tot
    dve(lambda e: e.tensor_tensor(out=pad_start[:], in0=pad_end[:], in1=padded[:], op=ALU.subtract), [pad_end, padded], [pad_start])
    basev = M.pp[pi]
    dve(lambda e: e.tensor_tensor(out=basev[:], in0=incl[:], in1=M.cnt[:], op=ALU.subtract), [incl, M.cnt], [basev])
    dve(lambda e: e.tensor_tensor(out=basev[:], in0=basev[:], in1=pad_start[:].unsqueeze(1).broadcast_to([128, NS, 16]), op=ALU.add),
        [basev, pad_start], [basev])
    basef = basev[:].rearrange("p s e -> p (s e)")
    for hh in range(2):
        dve(lambda e, hh=hh: e.tensor_tensor(out=basef[:, hh * 512:(hh + 1) * 512], in0=rank[hh][:, :], in1=basef[:, hh * 512:(hh + 1) * 512], op=ALU.add),
            [rank[hh], basev], [basev])
    dve(lambda e: e.tensor_tensor(out=basev[:], in0=basev[:], in1=M.OH[:], op=ALU.mult), [basev, M.OH], [basev])
    dve(lambda e: e.tensor_reduce(out=M.dest[:], in_=basev[:], axis=AX.X, op=ALU.add), [basev], [M.dest])
    dve(lambda e: e.tensor_copy(out=M.desti[:], in_=M.dest[:]), [M.dest], [M.desti])
    dve(lambda e: e.tensor_tensor(out=M.cmp[:], in0=pad_end[:].unsqueeze(1).broadcast_to([128, NBLK, 16]),
                                  in1=k["thr"][:].unsqueeze(2).broadcast_to([128, NBLK, 16]), op=ALU.is_le), [pad_end, k["thr"]], [M.cmp])
    dve(lambda e: e.tensor_reduce(out=M.blke[:], in_=M.cmp[:], axis=AX.X, op=ALU.add), [M.cmp], [M.blke])
    dve(lambda e: e.tensor_scalar(out=M.blke[:], in0=M.blke[:], scalar1=15.0, scalar2=128.0, op0=ALU.min, op1=ALU.mult), [M.blke], [M.blke])
    neq = M.cmp[:].rearrange("p j e -> p (j e)")[:, 0:NBLK]
    S.op("pool", lambda e: e.memset(neq[:, 0:2], 1.0), reads=[], writes=[M.cmp])
    dve(lambda e: e.tensor_tensor(out=neq[:, 2:NBLK], in0=M.blke[:, 2:NBLK], in1=M.blke[:, 0:NBLK - 2], op=ALU.not_equal), [M.blke, M.cmp], [M.cmp])
    dve(lambda e: e.tensor_scalar(out=M.blke[:], in0=M.blke[:], scalar1=k["piota"][:, 0:1], scalar2=float(l * 2048) - OOB_IDX, op0=ALU.add, op1=ALU.add),
        [M.blke, k["piota"]], [M.blke])
    dve(lambda e: e.tensor_tensor(out=M.blke[:], in0=M.blke[:], in1=neq, op=ALU.mult), [M.blke, M.cmp], [M.blke])
    dve(lambda e: e.tensor_scalar(out=M.blke[:], in0=M.blke[:], scalar1=OOB_IDX, scalar2=None, op0=ALU.add), [M.blke], [M.blke])
    dve(lambda e: e.tensor_copy(out=M.idxw[:], in_=M.blke[:]), [M.blke], [M.idxw])
    dve(lambda e: e.tensor_copy(out=M.info[:, :, 0], in_=k["tokid"][:]), [k["tokid"]], [M.info])
    dve(lambda e: e.tensor_copy(out=M.info[:, :, 1], in_=M.Wt[:].bitcast(I32)), [M.Wt], [M.info])
    dve(lambda e: e.tensor_copy(out=M.info[:, :, 2], in_=k["aid"][:]), [k["aid"]], [M.info])
    S.load(M.asg.h.ap().rearrange("(p j) c -> p j c", j=NBLK), k["init3"][:], reads=[k["init3"]], writes=[M.asg])
    for sg_ in range(NS):
        S.dma("pool", lambda e, sg_=sg_: e.indirect_dma_start(
            out=M.asg.h[:, :], out_offset=bass.IndirectOffsetOnAxis(ap=M.desti[:, sg_:sg_ + 1], axis=0),
            in_=M.info[:, sg_, :], in_offset=None), reads=[M.desti, M.info], writes=[M.asg], conc=True)

    dump(S, f"LG{l}", M.LG, [128, NT, 20]); dump(S, f"Wt{l}", M.Wt, [128, 2 * NT]); dump(S, f"OH{l}", M.OH, [128, 2 * NT, 16])
    dump(S, f"dest{l}", M.dest, [128, 2 * NT]); dump(S, f"blke{l}", M.blke, [128, NBLK]); dump(S, f"padend{l}", pad_end, [128, 16])
    for b_ in range(2):
        dump(S, f"G{l}{b_}", C.G[b_], [128, 1024]); dump(S, f"Sh{l}{b_}", C.Sh[b_], [128, 1024]); dump(S, f"Gt{l}{b_}", C.Gt[b_], [128, 1024])
    w1v = ins["moe_w1"].ap().rearrange("l e (p k) f -> (l e p) (k f)", k=8)
    w3v = ins["moe_w3"].ap().rearrange("l e (p k) f -> (l e p) (k f)", k=8)
    w2v = ins["moe_w2"].ap().rearrange("l e (p c) d -> (l e p) (c d)", c=4)
    mk_blk = S.mark()
    M.W1 = Pool(S, 2, [128, 4096], F32, "W1")
    M.W3 = Pool(S, 2, [128, 4096], F32, "W3")
    M.W2 = Pool(S, 2, [128, 4096], F32, "W2")
    M.g = Pool(S, 2, [128, 512], F32, "gg")
    M.gT = Pool(S, 2, [128, 4, 128], F32, "gT")
    M.y = Pool(S, 2, [128, 1024], F32, "yy")
    st = {}

    def stage_A(j):
        ab = M.ab.next()
        S.load(ab[:], M.asg.h[j * 128:(j + 1) * 128, :], reads=[M.asg], writes=[ab])
        xg = M.xg.next()
        S.dma("pool", lambda e, ab=ab, xg=xg: e.indirect_dma_start(
            out=xg[:], out_offset=None, in_=M.hbuf.h[:, :],
            in_offset=bass.IndirectOffsetOnAxis(ap=ab[:, 0:1], axis=0)), reads=[ab, M.hbuf], writes=[xg])
        Ws = []
        for wv, pool, nm in ((w1v, M.W1, "moe_w1"), (w3v, M.W3, "moe_w3"), (w2v, M.W2, "moe_w2")):
            W = pool.next()
            S.dma("pool", lambda e, W=W, wv=wv, j=j: e.indirect_dma_start(
                out=W[:], out_offset=None, in_=wv,
                in_offset=bass.IndirectOffsetOnAxis(ap=M.idxw[:, j:j + 1], axis=0),
                bounds_check=bc_reg(S, e), oob_is_err=False), reads=[M.idxw, ins[nm]], writes=[W])
            Ws.append(W)
        xgT = M.xgT.next()
        transpose_to(S, C, xgT, lambda kk, xg=xg: (xg, xg[:].rearrange("t (p k) -> t k p", k=8)[:, kk, :]), 8)
        st[j] = dict(ab=ab, W=Ws, xgT=xgT)

    def stage_B(j):
        d = st[j]
        xgT = d["xgT"]
        W1, W3, W2 = d["W"]
        p1, p3 = C.psum.next(), C.psum.next()
        for kk in range(8):
            S.op("pe", lambda e, kk=kk, xgT=xgT, W1=W1, p1=p1: e.matmul(p1[:, :], xgT[:, kk, :], W1[:, kk * 512:(kk + 1) * 512],
                                                                  start=(kk == 0), stop=(kk == 7)), reads=[xgT, W1], writes=[p1])
        for kk in range(8):
            S.op("pe", lambda e, kk=kk, xgT=xgT, W3=W3, p3=p3: e.matmul(p3[:, :], xgT[:, kk, :], W3[:, kk * 512:(kk + 1) * 512],
                                                                  start=(kk == 0), stop=(kk == 7)), reads=[xgT, W3], writes=[p3])
        g = M.g.next()
        S.op("act", lambda e, g=g, p1=p1: e.activation(out=g[:], in_=p1[:, :], func=AF.Silu), reads=[p1], writes=[g])
        S.op("dve", lambda e, g=g, p3=p3: e.tensor_tensor(out=g[:], in0=g[:], in1=p3[:, :], op=ALU.mult), reads=[g, p3], writes=[g])
        d["g"] = g

    def stage_C(j):
        d = st[j]
        g = d["g"]
        gT = M.gT.next()
        transpose_to(S, C, gT, lambda cc, g=g: (g, g[:].rearrange("t (p c) -> t c p", c=4)[:, cc, :]), 4)
        d["gT"] = gT

    def stage_D(j):
        d = st.pop(j)
        gT, ab = d["gT"], d["ab"]
        W2 = d["W"][2]
        y = M.y.next()
        for hh in range(2):
            py = C.psum.next()
            for cc in range(4):
                S.op("pe", lambda e, cc=cc, hh=hh, gT=gT, W2=W2, py=py: e.matmul(
                    py[:, :], gT[:, cc, :], W2[:, cc * 1024 + hh * 512: cc * 1024 + (hh + 1) * 512],
                    start=(cc == 0), stop=(cc == 3)), reads=[gT, W2], writes=[py])
            S.op("act" if hh == 0 else "dve",
                 (lambda e, hh=hh, y=y, py=py, ab=ab: e.activation(out=y[:, 0:512], in_=py[:, :], func=AF.Identity, scale=ab[:, 1:2].bitcast(F32)))
                 if hh == 0 else
                 (lambda e, hh=hh, y=y, py=py, ab=ab: e.tensor_scalar(out=y[:, 512:1024], in0=py[:, :], scalar1=ab[:, 1:2].bitcast(F32),
                                                                   scalar2=None, op0=ALU.mult)),
                 reads=[py, ab], writes=[y], conc=(hh > 0))
        S.dma("pool", lambda e, ab=ab, y=y: e.indirect_dma_start(
            out=M.ybuf.h[:, :], out_offset=bass.IndirectOffsetOnAxis(ap=ab[:, 2:3], axis=0),
            in_=y[:], in_offset=None), reads=[ab, y], writes=[M.ybuf], conc=(j > 0))

    stage_A(0)
    for j in range(NBLK):
        stage_B(j)
        if j + 1 < NBLK:
            stage_A(j + 1)
        stage_C(j)
        stage_D(j)

    S.release(mk_blk)
    M.yy = Pool(S, 2, [128, 2, 1024], F32, "ycomb")
    for i in range(NT):
        b = i // TPS
        yy = M.yy.next()
        S.load(yy[:], M.ybuf.h[i * 256:(i + 1) * 256, :].rearrange("(t s) d -> t s d", s=2), reads=[M.ybuf], writes=[yy])
        xt = C.xpool.next()
        S.load(xt[:], xin.h[i * 128:(i + 1) * 128, :], reads=[xin.tiles[i]], writes=[xt])
        S.op("dve", lambda e, yy=yy: e.tensor_tensor(out=yy[:, 0, :], in0=yy[:, 0, :], in1=yy[:, 1, :], op=ALU.add), reads=[yy], writes=[yy])
        S.op("dve", lambda e, yy=yy, b=b: e.tensor_tensor(out=yy[:, 0, :], in0=yy[:, 0, :], in1=C.Gt[b][:], op=ALU.mult), reads=[yy, C.Gt[b]], writes=[yy])
        S.op("dve", lambda e, yy=yy, xt=xt: e.tensor_tensor(out=xt[:], in0=xt[:], in1=yy[:, 0, :], op=ALU.add), reads=[yy, xt], writes=[xt])
        S.load(xout.h[i * 128:(i + 1) * 128, :], xt[:], reads=[xt], writes=[xout.tiles[i]])
    S.release(mk_moe)


def TV(ap, res):
    return T(ap, res=res)


def outproj_phase(S, C, ins, w_out, obuf, xin, xout):
    mk = S.mark()
    Wo = S.sb([128, 8, 1024], F32, "Wo")
    S.load(Wo[:], w_out.rearrange("(k p) n -> p k n", p=128), reads=[], writes=[Wo])
    opool = Pool(S, 2, [128, 1024], F32, "ot")

    def stage_a(i):
        ot = opool.next()
        S.load(ot[:], obuf.h[i * 128:(i + 1) * 128, :], reads=[obuf], writes=[ot])
        oT = C.hTpool.next()
        transpose_to(S, C, oT, lambda jj: (ot, ot[:, jj * 128:(jj + 1) * 128]), 8)
        xt = C.xpool.next()
        S.load(xt[:], xin.h[i * 128:(i + 1) * 128, :], reads=[xin.tiles[i]], writes=[xt])
        return ot, oT, xt

    def stage_b(i, ot, oT, xt):
        b = i // TPS
        for hh in range(2):
            py = C.psum.next()
            for kk in range(8):
                S.op("pe", lambda e, kk=kk, hh=hh, py=py: e.matmul(py[:, :], oT[:, kk, :], Wo[:, kk, hh * 512:(hh + 1) * 512],
                                                                start=(kk == 0), stop=(kk == 7)), reads=[oT, Wo], writes=[py])
            S.op("dve", lambda e, hh=hh, py=py: e.tensor_tensor(out=ot[:, hh * 512:(hh + 1) * 512], in0=py[:, :],
                                                                in1=C.Gt[b][:, hh * 512:(hh + 1) * 512], op=ALU.mult),
                 reads=[py, C.Gt[b], oT], writes=[ot])
        S.op("dve", lambda e: e.tensor_tensor(out=xt[:], in0=xt[:], in1=ot[:], op=ALU.add), reads=[ot, xt], writes=[xt])
        S.load(xout.h[i * 128:(i + 1) * 128, :], xt[:], reads=[xt], writes=[xout.tiles[i]])

    nxt = stage_a(0)
    for i in range(NT):
        cur_ = nxt
        if i + 1 < NT:
            nxt = stage_a(i + 1)
        stage_b(i, *cur_)
    S.release(mk)


def hgrn_sublayer(S, C, ins, l, xin, xout):
    j = l // 2
    mod_tiles(S, C, ins, l, 0)
    k = C.k
    mk = S.mark()
    GT = 4
    if not hasattr(C, "obuf"):
        C.obuf = S.dram("obuf", [NTOK, D])
    obuf = C.obuf
    lbB = S.sb([128, 1024], F32, "lbB")
    omlB = S.sb([128, 1024], F32, "omlB")
    gainB = S.sb([128, 128], F32, "gainB")
    lbd = ins["hgrn_lower_bounds"]
    S.load(lbB[:], lbd[l:l + 1, :].broadcast_to([128, 1024]), reads=[lbd], writes=[lbB])
    S.load(omlB[:], lbd[0:1, :].broadcast_to([128, 1024]), reads=[lbd], writes=[omlB])
    S.load(gainB[:], ins["hgrn_out_gain"][j:j + 1, :].broadcast_to([128, 128]), reads=[ins["hgrn_out_gain"]], writes=[gainB])
    S.op("dve", lambda e: e.tensor_tensor(out=lbB[:], in0=lbB[:], in1=omlB[:], op=ALU.subtract), reads=[lbB, omlB], writes=[lbB])
    S.op("act", lambda e: e.activation(out=lbB[:], in_=lbB[:], func=AF.Sigmoid), reads=[lbB], writes=[lbB])
    S.op("dve", lambda e: e.tensor_scalar(out=omlB[:], in0=lbB[:], scalar1=-1.0, scalar2=1.0, op0=ALU.mult, op1=ALU.add), reads=[lbB], writes=[omlB])
    hTgp = Pool(S, 1, [128, 8, GT * 128], F32, "hTg")
    Wp = Pool(S, 3, [128, 8, 512], F32, "Wh")
    NBUF = 2
    HB = []
    for i_ in range(NBUF):
        d = {}
        d["z"] = S.sb([128, GT, 512], F32, f"z{i_}")
        for n in ("fb", "kb", "lf", "qs", "e1", "qt", "kt", "kh", "oall", "sqb"):
            d[n] = S.sb([128, GT, 128], F32, f"{n}{i_}")
        d["av"] = S.sb([128, GT * 4], F32, f"av{i_}")
        d["ss"] = S.sb([128, GT], F32, f"ss{i_}")
        HB.append(d)
    Sst = S.sb([128, 8, 128], F32, "Sst")
    Sres = [Res(f"S{h}") for h in range(8)]
    Spool = Pool(S, 9, [128, 128], F32, "Sp")
    qkTp = Pool(S, 2, [128, 256], F32, "qkT")
    Zp = Pool(S, 2, [128, 4, 128], F32, "Z")
    Ap = Pool(S, 2, [128, 128], F32, "A")
    Vp = Pool(S, 2, [128, 4, 128], F32, "Vb")
    w_in = ins["hgrn_w_in"]

    def bcT(t, h):
        return t[:, h * 128:(h + 1) * 128].unsqueeze(1).broadcast_to([128, GT, 128])

    ps_front = Pool(S, 4, None, tiles=C.psum.t[0:4])
    ps_o = Pool(S, 2, None, tiles=C.psum.t[4:6])
    ps_prep = Pool(S, 2, None, tiles=C.psum.t[6:8])

    def prep_group(b, grp):
        hTg = hTgp.next()
        for tt in range(GT):
            i = b * TPS + grp * GT + tt
            xt = C.xpool.next()
            S.load(xt[:], xin.h[i * 128:(i + 1) * 128, :], reads=[xin.tiles[i]], writes=[xt])
            h_ = norm_tile(S, C, xt, b)
            transpose_to(S, C, TV(hTg[:, :, tt * 128:(tt + 1) * 128], hTg.res),
                         lambda jj, h_=h_: (h_, h_[:, jj * 128:(jj + 1) * 128]), 8, pool=ps_prep)
        return hTg

    def load_W(h):
        W = Wp.next()
        Wres = [Res(f"W{q}") for q in range(4)]
        for q in range(4):
            S.load(W[:, :, q * 128:(q + 1) * 128],
                   w_in[j, :, q * 1024 + h * 128: q * 1024 + (h + 1) * 128].rearrange("(k p) n -> p k n", p=128),
                   reads=[w_in], writes=[W], conc=(q > 0))
        return W

    def prep_head(hTg, h, B, W):
        z, fb, kb, lf, qs, e1, qt, kt, kh, av = (B[n] for n in ("z", "fb", "kb", "lf", "qs", "e1", "qt", "kt", "kh", "av"))
        for tt in range(GT):
            pz = ps_prep.next()
            for kk in range(8):
                S.op("pe", lambda e, kk=kk, tt=tt, W=W, pz=pz: e.matmul(pz[:, :], hTg[:, kk, tt * 128:(tt + 1) * 128], W[:, kk, :],
                                                                      start=(kk == 0), stop=(kk == 7)), reads=[hTg, W], writes=[pz])
            S.op("act", lambda e, tt=tt, pz=pz: e.copy(out=z[:, tt, :], in_=pz[:, :]), reads=[pz], writes=[z], conc=(tt > 0))
        zq, zf, zi, zg = (z[:, :, q * 128:(q + 1) * 128] for q in range(4))
        S.op("act", lambda e: e.activation(out=fb[:], in_=zf, func=AF.Sigmoid), reads=[z], writes=[fb])
        S.op("act", lambda e: e.activation(out=qs[:], in_=zq, func=AF.Silu), reads=[z], writes=[qs])
        S.op("dve", lambda e: e.tensor_tensor(out=fb[:], in0=fb[:], in1=bcT(omlB, h), op=ALU.mult), reads=[fb, omlB], writes=[fb])
        S.op("dve", lambda e: e.tensor_tensor(out=fb[:], in0=fb[:], in1=bcT(lbB, h), op=ALU.add), reads=[fb, lbB], writes=[fb])
        S.op("dve", lambda e: e.tensor_scalar(out=kb[:], in0=fb[:], scalar1=-1.0, scalar2=1.0, op0=ALU.mult, op1=ALU.add), reads=[fb], writes=[kb])
        S.op("act", lambda e: e.activation(out=lf[:], in_=fb[:], func=AF.Ln), reads=[fb], writes=[lf])
        return lambda: prep_head2(B)

    def prep_head2(B):
        z, fb, kb, lf, qs, e1, qt, kt, kh, av = (B[n] for n in ("z", "fb", "kb", "lf", "qs", "e1", "qt", "kt", "kh", "av"))
        lff = lf[:].rearrange("p t d -> p (t d)")
        pc, pr = ps_prep.next(), ps_prep.next()
        S.op("pe", lambda e: e.matmul(pc[:, :], k["ltri"][:], lff, start=True, stop=True), reads=[k["ltri"], lf], writes=[pc])
        S.op("pe", lambda e: e.matmul(pr[:, :], k["urev"][:], lff, start=True, stop=True), reads=[k["urev"], lf], writes=[pr])
        e1f = e1[:].rearrange("p t d -> p (t d)")
        S.op("act", lambda e: e.activation(out=e1f, in_=pc[:, :], func=AF.Exp), reads=[pc], writes=[e1])
        S.op("dve", lambda e: e.tensor_tensor(out=qt[:], in0=qs[:], in1=e1[:], op=ALU.mult), reads=[qs, e1], writes=[qt])
        S.op("act", lambda e: e.activation(out=e1f, in_=pc[:, :], func=AF.Exp, scale=-1.0), reads=[pc, qt], writes=[e1])
        S.op("dve", lambda e: e.tensor_tensor(out=kt[:], in0=kb[:], in1=e1[:], op=ALU.mult), reads=[kb, e1], writes=[kt])
        S.op("act", lambda e: e.activation(out=e1f, in_=pr[:, :], func=AF.Exp), reads=[pr, kt], writes=[e1])
        S.op("dve", lambda e: e.tensor_tensor(out=kh[:], in0=kb[:], in1=e1[:], op=ALU.mult), reads=[kb, e1], writes=[kh])
        pl = ps_prep.next()
        for tt in range(GT):
            S.op("pe", lambda e, tt=tt: e.matmul(pl[:, tt * 4:(tt + 1) * 4], lf[:, tt, :], k["cm"][:], start=True, stop=True),
                 reads=[lf, k["cm"]], writes=[pl])
        S.op("act", lambda e: e.activation(out=av[:], in_=pl[:, 0:GT * 4], func=AF.Exp), reads=[pl], writes=[av])

    def tiles_head(b, grp, h, B, mid_hook=None):
        z, qt, kt, kh, av, oall, sqb, ss = (B[n] for n in ("z", "qt", "kt", "kh", "av", "oall", "sqb", "ss"))
        zi, zg = z[:, :, 256:384], z[:, :, 384:512]
        cur = {"S": TV(Sst[:, h, :], Sres[h])}

        def front(tt):
            pt = ps_front.next()
            S.op("pe", lambda e: e.transpose(pt[:, 0:128], qt[:, tt, :], k["ident"][:]), reads=[qt, k["ident"]], writes=[pt])
            S.op("pe", lambda e: e.transpose(pt[:, 128:256], kt[:, tt, :], k["ident"][:]), reads=[kt, k["ident"]], writes=[pt])
            qkT = qkTp.next()
            S.op("act", lambda e: e.copy(out=qkT[:], in_=pt[:, 0:256]), reads=[pt], writes=[qkT])
            Z = Zp.next()
            S.op("dve", lambda e: e.tensor_tensor(out=Z[:], in0=pt[:, 0:128].unsqueeze(1).broadcast_to([128, 4, 128]),
                                                  in1=k["cmrow"][:].rearrange("p (c t) -> p c t", t=128), op=ALU.mult),
                 reads=[pt, k["cmrow"]], writes=[Z])
            pa = pt
            S.op("pe", lambda e: e.matmul(pa[:, 256:384], qkT[:, 128:256], qkT[:, 0:128], start=True, stop=True), reads=[qkT], writes=[pa])
            A = Ap.next()
            S.op("dve", lambda e: e.tensor_tensor(out=A[:], in0=pa[:, 256:384], in1=k["ltri"][:], op=ALU.mult), reads=[pa, k["ltri"]], writes=[A])
            Vb = Vp.next()
            S.op("pool", lambda e: e.tensor_tensor(out=Vb[:], in0=zi[:, tt, :].unsqueeze(1).broadcast_to([128, 4, 128]),
                                                   in1=k["cm"][:].unsqueeze(2).broadcast_to([128, 4, 128]), op=ALU.mult),
                 reads=[z, k["cm"]], writes=[Vb])
            pkv = ps_front.next()
            S.op("pe", lambda e: e.matmul(pkv[:, :], kh[:, tt, :], Vb[:].rearrange("p c e -> p (c e)"), start=True, stop=True),
                 reads=[kh, Vb], writes=[pkv])
            Ss = []
            for c in range(4):
                Scur = cur["S"]
                Ss.append(Scur)
                Sn = Spool.next() if not (tt == GT - 1 and c == 3) else TV(Sst[:, h, :], Sres[h])
                S.op("dve", lambda e, c=c, Sn=Sn, Scur=Scur: e.scalar_tensor_tensor(
                    out=Sn[:], in0=Scur[:], scalar=av[:, tt * 4 + c:tt * 4 + c + 1], in1=pkv[:, c * 128:(c + 1) * 128],
                    op0=ALU.mult, op1=ALU.add), reads=[Scur, av, pkv], writes=[Sn])
                cur["S"] = Sn
            return dict(Z=Z, A=A, Ss=Ss)

        def back(tt, d):
            Z, A, Ss = d["Z"], d["A"], d["Ss"]
            po = ps_o.next()
            S.op("pe", lambda e: e.matmul(po[:, 0:128], A[:], zi[:, tt, :], start=True, stop=False), reads=[A, z], writes=[po])
            for c in range(4):
                Scur = Ss[c]
                S.op("pe", lambda e, c=c, Scur=Scur: e.matmul(po[:, 0:128], Z[:, c, :], Scur[:], start=False, stop=(c == 3)),
                     reads=[Z, Scur], writes=[po])
            S.op("act", lambda e: e.copy(out=oall[:, tt, :], in_=po[:, 0:128]), reads=[po], writes=[oall], conc=(tt > 0))

        d = front(0)
        for tt in range(GT):
            dn = front(tt + 1) if tt + 1 < GT else None
            back(tt, d)
            d = dn
            if tt == 0 and mid_hook is not None:
                mid_hook()
        S.op("act", lambda e: e.activation(out=sqb[:], in_=oall[:], func=AF.Square), reads=[oall], writes=[sqb])
        S.op("dve", lambda e: e.tensor_reduce(out=ss[:], in_=sqb[:], axis=AX.X, op=ALU.add), reads=[sqb], writes=[ss])
        S.op("act", lambda e: e.activation(out=ss[:], in_=ss[:], func=AF.Sqrt, bias=EPS, scale=1.0 / 128), reads=[ss], writes=[ss])
        S.op("dve", lambda e: e.reciprocal(out=ss[:], in_=ss[:]), reads=[ss], writes=[ss])
        S.op("dve", lambda e: e.tensor_tensor(out=oall[:], in0=oall[:], in1=ss[:].unsqueeze(2).broadcast_to([128, GT, 128]), op=ALU.mult),
             reads=[oall, ss], writes=[oall])
        S.op("dve", lambda e: e.tensor_tensor(out=oall[:], in0=oall[:], in1=gainB[:].unsqueeze(1).broadcast_to([128, GT, 128]), op=ALU.mult),
             reads=[oall, gainB], writes=[oall])
        S.op("act", lambda e: e.activation(out=sqb[:], in_=zg, func=AF.Silu), reads=[z], writes=[sqb])
        S.op("dve", lambda e: e.tensor_tensor(out=oall[:], in0=oall[:], in1=sqb[:], op=ALU.mult), reads=[oall, sqb], writes=[oall])
        r0 = (b * TPS + grp * GT) * 128
        S.load(obuf.h[r0:r0 + GT * 128, h * 128:(h + 1) * 128].rearrange("(t p) e -> p t e", p=128), oall[:],
               reads=[oall], writes=[obuf], conc=not (b == 0 and grp == 0 and h == 0))

    work = [(b, grp, h) for b in range(NB) for grp in range(TPS // GT) for h in range(8)]
    hT_of = {}
    nb = 0

    Wq = {}

    def do_loadW(idx):
        if idx < len(work) and idx not in Wq:
            Wq[idx] = load_W(work[idx][2])

    def do_prep(idx):
        b, grp, h = work[idx]
        if h == 0:
            hT_of[(b, grp)] = prep_group(b, grp)
        return prep_head(hT_of[(b, grp)], h, HB[idx % NBUF], Wq.pop(idx))

    do_loadW(0)
    do_loadW(1)
    p2 = do_prep(0)
    p2()
    for idx, (b, grp, h) in enumerate(work):
        if grp == 0 and h == 0:
            S.op("pool", lambda e: e.memset(Sst[:], 0.0), writes=[Sst] + Sres)
        do_loadW(idx + 2)
        hook = do_prep(idx + 1) if idx + 1 < len(work) else None
        tiles_head(b, grp, h, HB[idx % NBUF], hook)
    S.release(mk)
    outproj_phase(S, C, ins, ins["hgrn_w_out"][j], obuf, xin, xout)


def dram_pitch(t, offset, pitch, rows, n):
    return AP(t.h.ap().tensor, offset, [[pitch, rows], [1, n]])


def nsa_sublayer(S, C, ins, l, xin, xout):
    j = l // 2
    mod_tiles(S, C, ins, l, 0)
    k = C.k
    ident = k["ident"]
    if not hasattr(C, "obuf"):
        C.obuf = S.dram("obuf", [NTOK, D])
    obuf = C.obuf
    hTbuf = S.dram("hTbuf", [NT, 128, 8, 128])
    LS, LW, LC = 2560, 1536, 4096
    TS = S.dram("tabS", [16, 128 * (LS + 1)])
    TW = S.dram("tabW", [16, 128 * (LW + 1)])
    TC = S.dram("tabC", [16, 128 * (LC + 16) + LC])
    mk_phase = S.mark()

    def n1_a(i):
        xt = C.xpool.next()
        S.load(xt[:], xin.h[i * 128:(i + 1) * 128, :], reads=[xin.tiles[i]], writes=[xt])
        return norm_tile(S, C, xt, i // TPS)

    def n1_b(i, h_):
        hT = C.hTpool.next()
        transpose_to(S, C, hT, lambda jj: (h_, h_[:, jj * 128:(jj + 1) * 128]), 8)
        S.load(hTbuf.h[i], hT[:], reads=[hT], writes=[hTbuf], conc=(i > 0))

    hn = n1_a(0)
    for i in range(NT):
        hc_ = hn
        if i + 1 < NT:
            hn = n1_a(i + 1)
        n1_b(i, hc_)

    mk_tab = S.mark()
    oh = S.sb([32, 2048], F32, "bkoh")
    rb = S.sb([32, 16], F32, "rb")
    BBs = [S.sb([128, LC + 16], F32, f"BB{i}") for i in range(2)]
    BWs = [S.sb([128, LW + 1], F32, f"BW{i}") for i in range(2)]
    rbbp = Pool(S, 2, [32, 128], F32, "rbb")
    S.load(oh[:], ins["bkoh"].ap(), reads=[ins["bkoh"]], writes=[oh])
    S.load(rb[:], ins["rel_bias"].ap(), reads=[ins["rel_bias"]], writes=[rb])
    for i_ in range(2):
        S.op("pool", lambda e, i_=i_: e.memset(BBs[i_][:], NEG), writes=[BBs[i_]])
        S.op("pool", lambda e, i_=i_: e.memset(BWs[i_][:], NEG), writes=[BWs[i_]])
    for h in range(16):
        BB, BW = BBs[h % 2], BWs[h % 2]
        rbb = rbbp.next()
        S.op("dve", lambda e, h=h, rbb=rbb: e.tensor_copy(out=rbb[:], in_=rb[:, h:h + 1].broadcast_to([32, 128])), reads=[rb], writes=[rbb])
        for c in range(4):
            pb = C.psum.next()
            S.op("pe", lambda e, c=c, rbb=rbb, pb=pb: e.matmul(pb[:, :], rbb[:], oh[:, c * 512:(c + 1) * 512], start=True, stop=True),
                 reads=[rbb, oh], writes=[pb])
            S.op("act", lambda e, c=c, pb=pb, BB=BB: e.copy(out=BB[:, 2048 + c * 512:2048 + (c + 1) * 512], in_=pb[:, :]),
                 reads=[pb], writes=[BB], conc=(c > 0))
        S.op("dve", lambda e, BB=BB, BW=BW: e.tensor_copy(out=BW[:, 0:1024], in_=BB[:, 1536:2560]), reads=[BB], writes=[BW])
        S.load(dram_pitch(TS, h * 128 * (LS + 1), LS + 1, 128, LS + 1), BB[:, 1536:1536 + LS + 1], reads=[BB], writes=[TS], conc=True)
        S.load(dram_pitch(TW, h * 128 * (LW + 1), LW + 1, 128, LW + 1), BW[:], reads=[BW], writes=[TW], conc=True)
        S.load(dram_pitch(TC, h * (128 * (LC + 16) + LC), LC + 16, 128, LC + 16), BB[:, :], reads=[BB], writes=[TC], conc=True)
    S.release(mk_tab)

    cmnf = S.sb([128, TPS, 32], F32, "cmnf")
    sadd = S.sb([128, TPS, 32], F32, "sadd")
    for nm, t_ in (("cmnf", cmnf), ("sadd", sadd)):
        S.load(t_[:], ins[nm].ap(), reads=[ins[nm]], writes=[t_])
    gain8 = S.sb([128, 8, 64], F32, "gain8")
    kg0B = S.sb([128, 64], F32, "kg0B")
    for q in range(4):
        S.load(gain8[:, q, :], ins["nsa_q_gain"][j:j + 1, :].broadcast_to([128, 64]), reads=[ins["nsa_q_gain"]], writes=[gain8], conc=(q > 0))
    for q in range(2):
        S.load(gain8[:, 4 + q, :], ins["nsa_k_gain"][j, 1 + q:2 + q, :].broadcast_to([128, 64]), reads=[ins["nsa_k_gain"]],
               writes=[gain8], conc=True)
    S.load(kg0B[:], ins["nsa_k_gain"][j, 0:1, :].broadcast_to([128, 64]), reads=[ins["nsa_k_gain"]], writes=[kg0B])
    S.op("dve", lambda e: e.tensor_scalar(out=gain8[:, 0:4, :], in0=gain8[:, 0:4, :], scalar1=0.125, scalar2=None, op0=ALU.mult),
         reads=[gain8], writes=[gain8])
    W1c = S.sb([128, 32, 64], F32, "W1c")
    peT = S.sb([128, 32], F32, "peT")
    W2c = S.sb([64, 2, 64], F32, "W2c")
    CST = S.sb([64, 2], F32, "CST")
    for a in range(2):
        S.load(W1c[a * 64:(a + 1) * 64, :, :], ins["nsa_cmp_w1"][j, a].rearrange("(l d) n -> d l n", d=64), reads=[ins["nsa_cmp_w1"]],
               writes=[W1c], conc=(a > 0))
        S.load(peT[a * 64:(a + 1) * 64, :], ins["nsa_cmp_pe"][j, a].rearrange("l d -> d l"), reads=[ins["nsa_cmp_pe"]], writes=[peT],
               conc=(a > 0), allow_slow_non_contiguous=True)
        S.load(W2c[:, a, :], ins["nsa_cmp_w2"][j, a], reads=[ins["nsa_cmp_w2"]], writes=[W2c], conc=(a > 0))
    for a in range(2):
        pb = C.psum.next()
        for l_ in range(32):
            S.op("pe", lambda e, a=a, l_=l_, pb=pb: e.matmul(pb[0:64, 0:1], W1c[a * 64:(a + 1) * 64, l_, :], peT[a * 64:(a + 1) * 64, l_:l_ + 1],
                                                          start=(l_ == 0), stop=(l_ == 31)), reads=[W1c, peT], writes=[pb])
        S.op("act", lambda e, a=a, pb=pb: e.copy(out=CST[:, a:a + 1], in_=pb[0:64, 0:1]), reads=[pb], writes=[CST], conc=(a > 0))
    w_in = ins["nsa_w_in"]
    exv = ins["ex"].ap().rearrange("s t k -> s (t k)")

    for b in range(NB):
        for g in range(4):
            mk_g = S.mark()
            ALLT = S.sb([128, 6, T_SEQ], F32, "ALLT")
            VA = S.sb([128, TPS, 2, 65], F32, "VA")
            GA = S.sb([128, TPS, 12], F32, "GA")
            KCN = S.sb([128, 127], F32, "KCN")
            VCA = S.sb([128, 97], F32, "VCA")
            oacc = S.sb([128, TPS, 4, 64], F32, "oacc")
            IMP = S.sb([128, TPS, 32], F32, "IMP")
            S.op("pool", lambda e, ALLT=ALLT: e.memset(ALLT[64:128, :, :], 0.0), writes=[ALLT])
            S.load(ALLT[64:96, 4, :], exv, reads=[ins["ex"]], writes=[ALLT], conc=True)
            S.op("pool", lambda e, KCN=KCN: e.memset(KCN[:], 0.0), writes=[KCN])
            S.op("pool", lambda e, VA=VA: e.memset(VA[:, :, :, 64:65], 1.0), writes=[VA])
            S.op("pool", lambda e, VCA=VCA: e.memset(VCA[:, 64:65], 1.0), writes=[VCA])
            S.load(VCA[0:127, 65:97], ins["c2s"].ap(), reads=[ins["c2s"]], writes=[VCA], conc=True)
            S.op("pool", lambda e, oacc=oacc: e.memset(oacc[:], 0.0), writes=[oacc])
            S.op("pool", lambda e, IMP=IMP: e.memset(IMP[:], 0.0), writes=[IMP])
            mk_p = S.mark()
            RAW = S.sb([128, 1, T_SEQ], F32, "RAW")
            Wg = S.sb([128, 8, 652], F32, "Wg")
            stgp = Pool(S, 2, [128, 8, 64], F32, "stg")
            sqn = S.sb([128, 384], F32, "sqn")
            st8p = Pool(S, 2, [128, 6], F32, "st6")
            cols = [(g * 256, 256)] + [(c0 + g * 64, 64) for c0 in (1536, 2048, 1024, 1280, 1792, 2304)] + [(2560 + 12 * g, 12)]
            off = 0
            for ci, (c0, wd) in enumerate(cols):
                S.load(Wg[:, :, off:off + wd], w_in[j, :, c0:c0 + wd].rearrange("(k p) n -> p k n", p=128),
                       reads=[w_in], writes=[Wg], conc=(ci > 0))
                off += wd
            ps_mm = Pool(S, 4, None, tiles=C.psum.t[0:4])
            ps_tr = Pool(S, 4, None, tiles=C.psum.t[4:8])

            def proj_mm(t):
                i = b * TPS + t
                hT = C.hTpool.next()
                S.load(hT[:], hTbuf.h[i], reads=[hTbuf], writes=[hT])
                pA, pB = ps_mm.next(), ps_mm.next()
                for kk in range(8):
                    S.op("pe", lambda e, kk=kk: e.matmul(pA[:, :], hT[:, kk, :], Wg[:, kk, 0:512], start=(kk == 0), stop=(kk == 7)),
                         reads=[hT, Wg], writes=[pA])
                for kk in range(8):
                    S.op("pe", lambda e, kk=kk: e.matmul(pB[:, 0:140], hT[:, kk, :], Wg[:, kk, 512:652], start=(kk == 0), stop=(kk == 7)),
                         reads=[hT, Wg], writes=[pB])
                return pA, pB

            def proj_post(t, pA, pB):
                st8 = st8p.next()
                stg = stgp.next()
                S.op("act", lambda e: e.activation(out=sqn[:], in_=pA[:, 0:384], func=AF.Square), reads=[pA], writes=[sqn])
                S.op("dve", lambda e: e.tensor_reduce(out=st8[:], in_=sqn[:].rearrange("p (a d) -> p a d", d=64), axis=AX.X, op=ALU.add),
                     reads=[sqn], writes=[st8])
                S.op("act", lambda e: e.activation(out=st8[:], in_=st8[:], func=AF.Sqrt, bias=EPS, scale=1.0 / 64), reads=[st8], writes=[st8])
                S.op("dve", lambda e: e.reciprocal(out=st8[:], in_=st8[:]), reads=[st8], writes=[st8])
                S.op("dve", lambda e: e.tensor_tensor(
                    out=stg[:, 0:6, :], in0=pA[:, 0:384].rearrange("p (a d) -> p a d", d=64), in1=st8[:].unsqueeze(2).broadcast_to([128, 6, 64]), op=ALU.mult),
                    reads=[pA, st8], writes=[stg])
                S.op("pool", lambda e: e.tensor_tensor(out=stg[:, 0:6, :], in0=stg[:, 0:6, :], in1=gain8[:, 0:6, :], op=ALU.mult),
                     reads=[stg, gain8], writes=[stg])
                S.op("act", lambda e: e.copy(out=stg[:, 6:8, :], in_=pA[:, 384:512].rearrange("p (a d) -> p a d", d=64)),
                     reads=[pA], writes=[stg], conc=True)
                S.op("act", lambda e: e.copy(out=VA[:, t, :, 0:64], in_=pB[:, 0:128].rearrange("p (a d) -> p a d", d=64)),
                     reads=[pB], writes=[VA], conc=True)
                S.op("act", lambda e: e.activation(out=GA[:, t, :], in_=pB[:, 128:140], func=AF.Sigmoid),
                     reads=[pB], writes=[GA], conc=(t > 0))
                tsl = slice(t * 128, (t + 1) * 128)
                transpose_to(S, C, TV(ALLT[:, :, tsl], ALLT.res), lambda jj: (stg, stg[:, jj, :]), 6, width=64, pool=ps_tr)
                transpose_to(S, C, TV(RAW[:, :, tsl], RAW.res),
                             lambda jj: (stg, stg[:, 6:8, :].rearrange("p a d -> p (a d)")), 1, pool=ps_tr)

            nxt = proj_mm(0)
            for t in range(TPS):
                cur_ = nxt
                if t + 1 < TPS:
                    nxt = proj_mm(t + 1)
                proj_post(t, *cur_)
            hid = S.sb([64, 2, 127], F32, "hid")
            kcn2 = S.sb([128, 64], F32, "kcn2")
            junk = S.sb([128, 64], F32, "junk")
            stc = S.sb([128, 2], F32, "stc")
            for a in range(2):
                ph = C.psum.next()
                for l_ in range(32):
                    S.op("pe", lambda e, a=a, l_=l_, ph=ph, RAW=RAW: e.matmul(ph[0:64, 0:127], W1c[a * 64:(a + 1) * 64, l_, :],
                                                                            RAW[a * 64:(a + 1) * 64, 0, l_:l_ + 16 * 126 + 1:16],
                                                                            start=(l_ == 0), stop=(l_ == 31)), reads=[W1c, RAW], writes=[ph])
                S.op("act", lambda e, a=a, ph=ph, hid=hid: e.activation(out=hid[:, a, :], in_=ph[0:64, 0:127], func=AF.Silu, bias=CST[:, a:a + 1]),
                     reads=[ph, CST], writes=[hid], conc=(a > 0))
            pk = C.psum.next()
            for a in range(2):
                S.op("pe", lambda e, a=a, pk=pk, hid=hid: e.matmul(pk[0:127, a * 64:(a + 1) * 64], hid[:, a, :], W2c[:, a, :], start=True, stop=True),
                     reads=[hid, W2c], writes=[pk])
            S.op("pool", lambda e, stc=stc: e.memset(stc[:], 0.0), writes=[stc])
            S.op("act", lambda e, pk=pk, junk=junk, stc=stc: e.activation(out=junk[0:127, :], in_=pk[0:127, 0:64], func=AF.Square, accum_out=stc[0:127, 0:1]),
                 reads=[pk, stc], writes=[junk, stc])
            S.op("act", lambda e, stc=stc: e.activation(out=stc[0:127, 1:2], in_=stc[0:127, 0:1], func=AF.Sqrt, bias=EPS, scale=1.0 / 64),
                 reads=[stc], writes=[stc])
            S.op("dve", lambda e, stc=stc: e.reciprocal(out=stc[0:127, 1:2], in_=stc[0:127, 1:2]), reads=[stc], writes=[stc])
            S.op("dve", lambda e, pk=pk, stc=stc, kcn2=kcn2: e.scalar_tensor_tensor(out=kcn2[0:127, :], in0=pk[0:127, 0:64], scalar=stc[0:127, 1:2],
                                                                                in1=kg0B[0:127, :], op0=ALU.mult, op1=ALU.mult),
                 reads=[pk, stc, kg0B], writes=[kcn2])
            S.op("act", lambda e, pk=pk, VCA=VCA: e.copy(out=VCA[0:127, 0:64], in_=pk[0:127, 64:128]), reads=[pk], writes=[VCA], conc=True)
            ptk = C.psum.next()
            S.op("pe", lambda e, ptk=ptk, kcn2=kcn2: e.transpose(ptk[0:64, 0:127], kcn2[0:127, :], ident[0:127, 0:127]), reads=[kcn2, ident], writes=[ptk])
            S.op("act", lambda e, ptk=ptk, KCN=KCN: e.copy(out=KCN[0:64, :], in_=ptk[0:64, 0:127]), reads=[ptk], writes=[KCN], conc=True)
            S.release(mk_p)
            tmpp = Pool(S, 4, [128, 512], F32, "tmp")
            Ep = Pool(S, 4, [128, 512], F32, "E")
            cfp = Pool(S, 16, [128, 2], F32, "cf")
            evp = Pool(S, 16, [128, 97], F32, "ev")
            sc = S.sb([128, TPS, 32], F32, "sc")
            M8 = S.sb([128, TPS, 8], F32, "M8")
            NS = S.sb([32, T_SEQ], F32, "NS")

            pending = []

            def epilogue(po_, t, r, br, ncol, with_imp):
                cf = cfp.next()
                po = evp.next()
                S.op("act", lambda e: e.copy(out=po[:, 0:ncol], in_=po_[:, 0:ncol]), reads=[po_], writes=[po])

                def math():
                    S.op("dve", lambda e: e.tensor_scalar(out=cf[:, 0:1], in0=po[:, 64:65], scalar1=1e-30, scalar2=None, op0=ALU.max),
                         reads=[po], writes=[cf])
                    S.op("dve", lambda e: e.reciprocal(out=cf[:, 0:1], in_=cf[:, 0:1]), reads=[cf], writes=[cf])
                    if with_imp:
                        S.op("dve", lambda e: e.scalar_tensor_tensor(out=IMP[:, t, :], in0=po[:, 65:97], scalar=cf[:, 0:1], in1=IMP[:, t, :],
                                                                     op0=ALU.mult, op1=ALU.add), reads=[po, cf, IMP], writes=[IMP])
                    S.op("dve", lambda e: e.tensor_tensor(out=cf[:, 1:2], in0=cf[:, 0:1], in1=GA[:, t, r * 3 + br:r * 3 + br + 1], op=ALU.mult),
                         reads=[cf, GA], writes=[cf])
                    S.op("dve", lambda e: e.scalar_tensor_tensor(out=oacc[:, t, r, :], in0=po[:, 0:64], scalar=cf[:, 1:2], in1=oacc[:, t, r, :],
                                                                 op0=ALU.mult, op1=ALU.add), reads=[po, cf, oacc], writes=[oacc])
                pending.append(math)

            def flush_pending(n=None):
                k_ = len(pending) if n is None else min(n, len(pending))
                for _ in range(k_):
                    pending.pop(0)()

            pspool = Pool(S, 3, None, tiles=[C.psum.t[0], C.psum.t[1], C.psum.t[6]])
            po4 = C.psum.t[2:6]
            cbank = C.psum.t[7]

            def col_range(kind, Q, kt):
                if kind == "s":
                    return max(0, kt - 4 * Q) * 128, 512
                i = kt - (4 * Q - 4)
                if i <= 3:
                    return 0, (i + 1) * 128
                return (i - 4) * 128, 512

            started = {}

            def a_score(job, r, tabs):
                kind, Q, br, kidx, kts, kt = job
                qsl = slice(Q * 512, (Q + 1) * 512)
                ps = pspool.next()
                tmp = tmpp.next()
                E = Ep.next()
                if kind == "c":
                    SC = tabs["c"]
                    S.op("pe", lambda e: e.matmul(ps[0:127, :], KCN[:, :], ALLT[:, r, qsl], start=True, stop=True), reads=[KCN, ALLT], writes=[ps])
                    S.op("dve", lambda e: e.tensor_tensor(out=tmp[0:127, :], in0=ps[0:127, :], in1=SC[0:127, qsl], op=ALU.add),
                         reads=[ps, SC], writes=[tmp])
                    S.op("act", lambda e: e.activation(out=E[0:127, :], in_=tmp[0:127, :], func=AF.Exp), reads=[tmp], writes=[E])
                else:
                    tab = tabs[kind]
                    c0, c1 = col_range(kind, Q, kt)
                    ksl = slice(kt * 128, (kt + 1) * 128)
                    S.op("pe", lambda e: e.matmul(ps[:, c0:c1], ALLT[:, kidx, ksl], ALLT[:, r, Q * 512 + c0:Q * 512 + c1], start=True, stop=True),
                         reads=[ALLT], writes=[ps])
                    m0 = Q * 512 - kt * 128 + 512
                    S.op("dve", lambda e: e.tensor_tensor(out=tmp[:, c0:c1], in0=ps[:, c0:c1], in1=tab[:, m0 + c0:m0 + c1], op=ALU.add),
                         reads=[ps, tab], writes=[tmp])
                    S.op("act", lambda e: e.activation(out=E[:, c0:c1], in_=tmp[:, c0:c1], func=AF.Exp), reads=[tmp], writes=[E])
                return E

            def a_pv(job, E, r):
                kind, Q, br, kidx, kts, kt = job
                if kind == "c":
                    for qs in range(4):
                        S.op("pe", lambda e, qs=qs: e.matmul(cbank[:, qs * 100:qs * 100 + 97], E[0:127, qs * 128:(qs + 1) * 128], VCA[0:127, 0:97],
                                                         start=True, stop=True), reads=[E, VCA], writes=[cbank])
                    for qs in range(4):
                        epilogue(TV(cbank[:, qs * 100:qs * 100 + 97], cbank.res), Q * 4 + qs, r, 0, 97, True)
                    return
                c0, c1 = col_range(kind, Q, kt)
                if kt == kts[0]:
                    for qs in range(4):
                        started[qs] = False
                for qs in range(4):
                    if not (c0 <= qs * 128 < c1):
                        continue
                    first = not started[qs]
                    started[qs] = True
                    S.op("pe", lambda e, po=po4[qs], qs=qs, first=first, last=(kt == 4 * Q + qs): e.matmul(
                        po[:, 0:65], E[:, qs * 128:(qs + 1) * 128], VA[:, kt, br - 1, :], start=first, stop=last),
                        reads=[E, VA], writes=[po4[qs]])
                if kt == kts[-1]:
                    for qs in range(4):
                        epilogue(po4[qs], Q * 4 + qs, r, br, 65, False)

            def run_jobs(jobs, r, tabs, LA=2):
                Es = [a_score(jobs[i], r, tabs) for i in range(min(LA, len(jobs)))]
                for ji, job in enumerate(jobs):
                    if ji + LA < len(jobs):
                        Es.append(a_score(jobs[ji + LA], r, tabs))
                    flush_pending(1)
                    a_pv(job, Es.pop(0), r)
                flush_pending()

            mk_c = S.mark()
            SC = S.sb([128, 2048], F32, "SC")
            SW = S.sb([128, LW], F32, "SW")
            for r in range(4):
                h = 4 * g + r
                S.load(SC[:, :], dram_pitch(TC, h * (128 * (LC + 16) + LC) + 2017, LC, 128, 2048), reads=[TC], writes=[SC])
                S.load(SW[:], dram_pitch(TW, h * 128 * (LW + 1), LW, 128, LW), reads=[TW], writes=[SW])
                jobs = []
                for Q in range(4):
                    jobs.append(("c", Q, 0, None, None, None))
                    kts = list(range(max(0, 4 * Q - 4), 4 * Q + 4))
                    for kt in kts:
                        jobs.append(("w", Q, 2, 5, kts, kt))
                run_jobs(jobs, r, {"c": SC, "w": SW})
            S.release(mk_c)
            S.op("dve", lambda e: e.tensor_tensor(out=sc[:], in0=IMP[:], in1=cmnf[:], op=ALU.mult), reads=[IMP, cmnf], writes=[sc])
            S.op("dve", lambda e: e.tensor_tensor(out=sc[:], in0=sc[:], in1=sadd[:], op=ALU.add), reads=[sc, sadd], writes=[sc])
            for t in range(TPS):
                S.op("dve", lambda e, t=t: e.max(out=M8[:, t, :], in_=sc[:, t, :]), reads=[sc], writes=[M8], conc=(t > 0))
            S.op("dve", lambda e: e.tensor_tensor(out=sc[:], in0=sc[:], in1=M8[:, :, 7:8].broadcast_to([128, TPS, 32]), op=ALU.is_ge),
                 reads=[sc, M8], writes=[sc])
            S.op("dve", lambda e: e.tensor_scalar(out=sc[:], in0=sc[:], scalar1=-1.0, scalar2=-NEG, op0=ALU.add, op1=ALU.mult), reads=[sc], writes=[sc])
            for t4 in range(4):
                pb = C.psum.next()
                for q in range(4):
                    S.op("pe", lambda e, pb=pb, q=q, t4=t4: e.transpose(pb[0:32, q * 128:(q + 1) * 128], sc[:, t4 * 4 + q, :], ident[:]),
                         reads=[sc, ident], writes=[pb])
                S.op("act", lambda e, pb=pb, t4=t4: e.copy(out=NS[:, t4 * 512:(t4 + 1) * 512], in_=pb[0:32, :]), reads=[pb], writes=[NS], conc=(t4 > 0))
            for r in range(4):
                S.load(ALLT[64:96, r, :], NS[:], reads=[NS], writes=[ALLT], conc=(r > 0))
            SS = S.sb([128, LS], F32, "SS")
            for r in range(4):
                h = 4 * g + r
                S.load(SS[:], dram_pitch(TS, h * 128 * (LS + 1), LS, 128, LS), reads=[TS], writes=[SS])
                jobs = []
                for Q in range(4):
                    kts = list(range(0, 4 * Q + 4))
                    for kt in kts:
                        jobs.append(("s", Q, 1, 4, kts, kt))
                run_jobs(jobs, r, {"s": SS})
            r0 = b * T_SEQ
            S.load(obuf.h[r0:r0 + T_SEQ, g * 256:(g + 1) * 256].rearrange("(t p) c -> p t c", p=128), oacc[:].rearrange("p t r d -> p t (r d)"),
                   reads=[oacc], writes=[obuf], conc=not (b == 0 and g == 0))
            S.release(mk_g)
    S.release(mk_phase)
    outproj_phase(S, C, ins, ins["nsa_w_out"][j], obuf, xin, xout)


class XBuf:
    def __init__(self, t):
        self.h = t.h
        self.t = t
        self.tiles = [Res(f"xt{i}") for i in range(NT)]


W_SHAPES = {
    "ada_w": [2, 2, 1024, 3072], "ada_b": [2, 2, 3072], "norm_g": [2, 2, 1024], "rel_bias": [32, 16],
    "nsa_w_in": [1, 1024, 2608], "nsa_q_gain": [1, 64], "nsa_k_gain": [1, 3, 64], "nsa_cmp_pe": [1, 2, 32, 64],
    "nsa_cmp_w1": [1, 2, 2048, 64], "nsa_cmp_w2": [1, 2, 64, 64], "nsa_w_out": [1, 1024, 1024],
    "hgrn_w_in": [1, 1024, 4096], "hgrn_lower_bounds": [2, 1024], "hgrn_out_gain": [1, 128],
    "hgrn_w_out": [1, 1024, 1024], "moe_router_group": [2, 1024, 4], "moe_router_expert": [2, 1024, 16],
    "moe_w1": [2, 16, 1024, 512], "moe_w3": [2, 16, 1024, 512], "moe_w2": [2, 16, 512, 1024],
}

ALL_SUBLAYERS = [(0, 0), (0, 1), (1, 0), (1, 1)]


def build(sublayers=ALL_SUBLAYERS):
    nc = bass.Bass("TRN2", target_bir_lowering=False)
    S = Sch(nc)
    C = Ctx()
    ins = {}
    ins["x"] = S.dram("x", [NTOK, D], kind="ExternalInput")
    ins["c"] = S.dram("c", [NB, D], kind="ExternalInput")
    for name, shp in W_SHAPES.items():
        ins[name] = S.dram(name, shp, kind="ExternalInput")
    for name, arr in host_consts().items():
        ins[name] = S.dram(name, list(arr.shape), I32 if arr.dtype == np.int32 else F32, kind="ExternalInput")
    out = XBuf(S.dram("out", [NTOK, D], kind="ExternalOutput"))
    scr = [XBuf(S.dram(f"xs{i}", [NTOK, D])) for i in range(2)]
    setup_common(S, C, ins)
    cur = XBuf(ins["x"])
    n = len(sublayers)
    for i, (l, s) in enumerate(sublayers):
        dst = out if i == n - 1 else scr[i % 2]
        if s == 1:
            moe_sublayer(S, C, ins, l, cur, dst)
        elif l % 2 == 0:
            nsa_sublayer(S, C, ins, l, cur, dst)
        else:
            hgrn_sublayer(S, C, ins, l, cur, dst)
        cur = dst
    S.finish()
    S.emit()
    return nc


def kernel(**inputs):
    inputs = {k: np.asarray(v) for k, v in inputs.items()}
    nc = build()
    hc = host_consts()
    x = np.ascontiguousarray(inputs["x"], dtype=np.float32)
    c = np.ascontiguousarray(inputs["c"], dtype=np.float32)
    in_maps = []
    for core in range(N_CORES):
        m = {"x": x[core * NB:(core + 1) * NB].reshape(NTOK, D), "c": c[core * NB:(core + 1) * NB]}
        for name in W_SHAPES:
            m[name] = np.ascontiguousarray(inputs[name], dtype=np.float32)
        m.update(hc)
        in_maps.append(m)
    res = run_bass_kernel_spmd(nc, in_maps, core_ids=list(range(N_CORES)))
    outs = [r["out"].reshape(NB, T_SEQ, D) for r in res.results]
    return np.concatenate(outs, axis=0).astype(np.float32)
```
